# Optimizing a Trainium2 kernel written in Bass

```python
import jax, jax.numpy as jnp
from jax import lax
import numpy as np

D_MODEL = 2048
BATCH = 8
SEQ = 2048
DEPTH = 2

GRID_W = 64
CTX_LEN = 256
EPS = 1e-6
NEG = -1e30
N_EVEN = (DEPTH + 1) // 2
N_ODD = DEPTH // 2

NA_HEADS = 8
NA_HEAD_DIM = 128
NA_WIDTH = NA_HEADS * NA_HEAD_DIM
NA_WIN_H = 8
NA_WIN_W = 16
SG_GROUPS = 8
SG_WIDTH = D_MODEL // 2
SG_GROUP_DIM = SG_WIDTH // SG_GROUPS
SG_CHUNK = 128
AB_IN = 3 * NA_WIDTH + 2 * SG_WIDTH
AB_OUT = NA_WIDTH + SG_WIDTH
MLA_HEADS = 16
Q_LORA = 512
KV_LORA = 256
QK_NOPE = 128
QK_ROPE = 64
V_DIM = 128
QK_DIM = QK_NOPE + QK_ROPE
MLA_IN = Q_LORA + KV_LORA + QK_ROPE
Q_BLOCK = 128
ROPE_THETA = 10000.0
N_EXPERTS = 32
TOP_K = 4
D_FF = D_MODEL
SWIGLU_ALPHA = 1.702
SWIGLU_LIMIT = 7.0
MOE_BLOCK = 128

kernel_name = "hybrid_na_gmlp_mla_moe_diffusion_trunk"


def rms_norm(x, g):
    xf = x.astype(jnp.float32)
    y = xf * lax.rsqrt(jnp.mean(xf * xf, axis=-1, keepdims=True) + EPS)
    return (y * g.astype(jnp.float32)).astype(x.dtype)


def layer_norm(x, g, b):
    xf = x.astype(jnp.float32)
    mu = jnp.mean(xf, axis=-1, keepdims=True)
    var = jnp.mean(jnp.square(xf - mu), axis=-1, keepdims=True)
    y = (xf - mu) * lax.rsqrt(var + EPS) * g.astype(jnp.float32) + b.astype(jnp.float32)
    return y.astype(x.dtype)


def modulate(x, shift, scale):
    return x * (1 + scale) + shift


def axial_rope_tables(n_tokens, dim):
    t = jnp.arange(n_tokens)
    row = (t // GRID_W).astype(jnp.float32)
    col = (t % GRID_W).astype(jnp.float32)
    n_freq = dim // 4
    inv = ROPE_THETA ** (-jnp.arange(n_freq, dtype=jnp.float32) / n_freq)
    ang = jnp.concatenate([row[:, None] * inv, col[:, None] * inv], axis=-1)
    return jnp.cos(ang), jnp.sin(ang)


def apply_rope(x, cos, sin):
    x1, x2 = jnp.split(x, 2, axis=-1)
    c = cos[:, None, :].astype(x.dtype)
    s = sin[:, None, :].astype(x.dtype)
    return jnp.concatenate([x1 * c - x2 * s, x1 * s + x2 * c], axis=-1)


def dense_attention(q, k, v):
    s = jnp.einsum('bqhd,bkhd->bhqk', q, k).astype(jnp.float32) * (q.shape[-1] ** -0.5)
    p = jax.nn.softmax(s, axis=-1)
    return jnp.einsum('bhqk,bkhd->bqhd', p.astype(v.dtype), v)


def blocked_attention(q, k, v):
    B, S, H, dq = q.shape
    n_blk = S // Q_BLOCK
    qb = jnp.moveaxis(q.reshape(B, n_blk, Q_BLOCK, H, dq), 1, 0)
    out = lax.map(lambda qi: dense_attention(qi, k, v), qb)
    return jnp.moveaxis(out, 0, 1).reshape(B, S, H, v.shape[-1])


def neighbourhood_attention(q, k, v, kc, vc, rel_bias):
    B, S, H, d = q.shape
    rows = S // GRID_W
    kh = min(NA_WIN_H, rows)
    kw = NA_WIN_W
    scale = d ** -0.5
    kg = k.reshape(B, rows, GRID_W, H, d)
    vg = v.reshape(B, rows, GRID_W, H, d)
    col = jnp.arange(GRID_W)
    col_start = jnp.clip(col - kw // 2, 0, GRID_W - kw)
    col_mask = (col[None, :] >= col_start[:, None]) & (col[None, :] < col_start[:, None] + kw)
    mask = jnp.broadcast_to(col_mask[:, None, :], (GRID_W, kh, GRID_W)).reshape(GRID_W, kh * GRID_W)
    col_idx = jnp.clip(col[None, :] - col[:, None] + NA_WIN_W - 1, 0, 2 * NA_WIN_W - 2)

    def row_block(args):
        r, q_r = args
        r0 = jnp.clip(r - kh // 2, 0, rows - kh)
        k_band = lax.dynamic_slice_in_dim(kg, r0, kh, axis=1).reshape(B, kh * GRID_W, H, d)
        v_band = lax.dynamic_slice_in_dim(vg, r0, kh, axis=1).reshape(B, kh * GRID_W, H, d)
        row_idx = r0 + jnp.arange(kh) - r + NA_WIN_H - 1
        bias = rel_bias[:, row_idx[:, None, None], col_idx[None, :, :]]
        bias = jnp.transpose(bias, (0, 2, 1, 3)).reshape(H, GRID_W, kh * GRID_W).astype(jnp.float32)
        s_loc = jnp.einsum('bqhd,bkhd->bhqk', q_r, k_band).astype(jnp.float32) * scale + bias
        s_loc = jnp.where(mask, s_loc, NEG)
        s_ctx = jnp.einsum('bqhd,bchd->bhqc', q_r, kc).astype(jnp.float32) * scale
        p = jax.nn.softmax(jnp.concatenate([s_loc, s_ctx], axis=-1), axis=-1).astype(v.dtype)
        n_loc = kh * GRID_W
        return (jnp.einsum('bhqk,bkhd->bqhd', p[..., :n_loc], v_band)
                + jnp.einsum('bhqc,bchd->bqhd', p[..., n_loc:], vc))

    q_rows = jnp.moveaxis(q.reshape(B, rows, GRID_W, H, d), 1, 0)
    out = lax.map(row_block, (jnp.arange(rows), q_rows))
    return jnp.moveaxis(out, 0, 1).reshape(B, S, H, d)


def spatial_gating(u, z, w_s, b_s):
    B, T, _ = u.shape
    n = T // SG_CHUNK
    zg = z.reshape(B, n, SG_CHUNK, SG_GROUPS, SG_GROUP_DIM)
    mixed = jnp.einsum('gpq,bnqgc->bnpgc', w_s, zg) + b_s.T[None, None, :, :, None]
    return u * mixed.reshape(B, T, SG_WIDTH)


def mixer_ab(h, hc, w_in, w_out, q_g, k_g, rel_bias, sg_norm_g, sg_norm_b, sg_w, sg_b, ctx_out):
    B, S, _ = h.shape
    Bc, T, _ = hc.shape

    def heads(p):
        return p.reshape(*p.shape[:-1], NA_HEADS, NA_HEAD_DIM)

    def gated_mix(p_uv):
        u, z = jnp.split(jax.nn.gelu(p_uv), 2, axis=-1)
        return spatial_gating(u, layer_norm(z, sg_norm_g, sg_norm_b), sg_w, sg_b)

    p = h @ w_in
    q = rms_norm(heads(p[..., :NA_WIDTH]), q_g)
    k = rms_norm(heads(p[..., NA_WIDTH:2 * NA_WIDTH]), k_g)
    v = heads(p[..., 2 * NA_WIDTH:3 * NA_WIDTH])
    if ctx_out:
        pc = hc @ w_in
        qc = rms_norm(heads(pc[..., :NA_WIDTH]), q_g)
        kc = rms_norm(heads(pc[..., NA_WIDTH:2 * NA_WIDTH]), k_g)
        vc = heads(pc[..., 2 * NA_WIDTH:3 * NA_WIDTH])
    else:
        pkv = hc @ w_in[:, NA_WIDTH:3 * NA_WIDTH]
        kc = rms_norm(heads(pkv[..., :NA_WIDTH]), k_g)
        vc = heads(pkv[..., NA_WIDTH:])
    a_out = neighbourhood_attention(q, k, v, kc, vc, rel_bias).reshape(B, S, NA_WIDTH)
    y = jnp.concatenate([a_out, gated_mix(p[..., 3 * NA_WIDTH:])], axis=-1) @ w_out
    if not ctx_out:
        return y, None
    ac = dense_attention(qc, kc, vc).reshape(Bc, T, NA_WIDTH)
    yc = jnp.concatenate([ac, gated_mix(pc[..., 3 * NA_WIDTH:])], axis=-1) @ w_out
    return y, yc


def mixer_mla(h, hc, cos, sin, w_in, q_norm_g, kv_norm_g, w_uq, w_ukv, q_g, k_g, w_out, ctx_out):
    B, S, _ = h.shape
    Bc, T, _ = hc.shape

    def rope_tail(t):
        return jnp.concatenate([t[..., :QK_NOPE], apply_rope(t[..., QK_NOPE:], cos, sin)], axis=-1)

    def queries(p_q, rope):
        c_q = rms_norm(p_q, q_norm_g)
        q = (c_q @ w_uq).reshape(*p_q.shape[:2], MLA_HEADS, QK_DIM)
        q = rms_norm(q, q_g)
        return rope_tail(q) if rope else q

    def keys_values(p_kv, rope):
        c_kv = rms_norm(p_kv[..., :KV_LORA], kv_norm_g)
        k_pe = p_kv[..., KV_LORA:]
        kv = (c_kv @ w_ukv).reshape(*p_kv.shape[:2], MLA_HEADS, QK_NOPE + V_DIM)
        k_pe = jnp.broadcast_to(k_pe[:, :, None, :], (*p_kv.shape[:2], MLA_HEADS, QK_ROPE))
        k = rms_norm(jnp.concatenate([kv[..., :QK_NOPE], k_pe], axis=-1), k_g)
        return (rope_tail(k) if rope else k), kv[..., QK_NOPE:]

    p = h @ w_in
    q = queries(p[..., :Q_LORA], True)
    k, v = keys_values(p[..., Q_LORA:], True)
    if ctx_out:
        pc = hc @ w_in
        qc = queries(pc[..., :Q_LORA], False)
        kc, vc = keys_values(pc[..., Q_LORA:], False)
    else:
        kc, vc = keys_values(hc @ w_in[:, Q_LORA:], False)
    k_all = jnp.concatenate([kc, k], axis=1)
    v_all = jnp.concatenate([vc, v], axis=1)
    y = blocked_attention(q, k_all, v_all).reshape(B, S, MLA_HEADS * V_DIM) @ w_out
    if not ctx_out:
        return y, None
    yc = dense_attention(qc, kc, vc).reshape(Bc, T, MLA_HEADS * V_DIM) @ w_out
    return y, yc


def moe_ffn(t, w_router, b_router, w_gu, b_gu, w_down, b_down):
    N, D = t.shape
    logits = (t @ w_router).astype(jnp.float32) + b_router.astype(jnp.float32)
    top_val, top_idx = lax.top_k(logits, TOP_K)
    gate = jax.nn.softmax(top_val, axis=-1)
    nk = N * TOP_K
    flat_e = top_idx.reshape(nk)
    order = jnp.argsort(flat_e)
    sorted_e = flat_e[order]
    counts = jnp.bincount(flat_e, length=N_EXPERTS)
    padded = (counts + MOE_BLOCK - 1) // MOE_BLOCK * MOE_BLOCK
    pad_end = jnp.cumsum(padded)
    pad_start = pad_end - padded
    start = jnp.cumsum(counts) - counts
    dest = pad_start[sorted_e] + jnp.arange(nk) - start[sorted_e]
    n_blocks = -(-nk // MOE_BLOCK) + N_EXPERTS
    tok_sorted = order // TOP_K
    rows_tok = jnp.zeros((n_blocks * MOE_BLOCK,), jnp.int32).at[dest].set(tok_sorted.astype(jnp.int32))
    block_e = jnp.minimum(jnp.searchsorted(pad_end, jnp.arange(n_blocks) * MOE_BLOCK, side='right'), N_EXPERTS - 1)

    def expert_block(args):
        e, tok = args
        gu = t[tok] @ w_gu[e] + b_gu[e]
        g, u = jnp.split(gu, 2, axis=-1)
        g = jnp.minimum(g, SWIGLU_LIMIT)
        u = jnp.clip(u, -SWIGLU_LIMIT, SWIGLU_LIMIT)
        hid = (u + 1) * (g * jax.nn.sigmoid(SWIGLU_ALPHA * g))
        return hid @ w_down[e] + b_down[e]

    out = lax.map(expert_block, (block_e, rows_tok.reshape(n_blocks, MOE_BLOCK)))
    y_sorted = out.reshape(n_blocks * MOE_BLOCK, D)[dest]
    w_sorted = gate.reshape(nk)[order].astype(t.dtype)
    return jax.ops.segment_sum(y_sorted * w_sorted[:, None], tok_sorted, num_segments=N)


def setup_inputs(seed: int = 0) -> dict:
    key = jax.random.key(seed)
    ks = jax.random.split(key, 40)
    f32 = jnp.float32

    def nrm(k, shape, scale):
        return jax.random.normal(k, shape, f32) * scale

    def gain(k, shape):
        return 1.0 + 0.02 * jax.random.normal(k, shape, f32)

    D = D_MODEL
    return {
        "x": nrm(ks[0], (BATCH, SEQ, D), 1.0),
        "c": nrm(ks[1], (BATCH, D), 1.0),
        "ctx": nrm(ks[2], (BATCH, CTX_LEN, D), 1.0),
        "c_ctx": nrm(ks[3], (D,), 1.0),
        "ada_w": nrm(ks[4], (DEPTH, D, 6 * D), 0.5 * D ** -0.5),
        "ada_b": nrm(ks[5], (DEPTH, 6 * D), 0.01),
        "norm1_g": gain(ks[6], (DEPTH, D)),
        "norm2_g": gain(ks[7], (DEPTH, D)),
        "ab_w_in": nrm(ks[8], (N_EVEN, D, AB_IN), D ** -0.5),
        "ab_w_out": nrm(ks[9], (N_EVEN, AB_OUT, D), AB_OUT ** -0.5),
        "na_q_g": gain(ks[10], (N_EVEN, NA_HEAD_DIM)),
        "na_k_g": gain(ks[11], (N_EVEN, NA_HEAD_DIM)),
        "na_rel_bias": nrm(ks[12], (N_EVEN, NA_HEADS, 2 * NA_WIN_H - 1, 2 * NA_WIN_W - 1), 0.1),
        "sg_norm_g": gain(ks[13], (N_EVEN, SG_WIDTH)),
        "sg_norm_b": nrm(ks[14], (N_EVEN, SG_WIDTH), 0.02),
        "sg_w": nrm(ks[15], (N_EVEN, SG_GROUPS, SG_CHUNK, SG_CHUNK), SG_CHUNK ** -0.5),
        "sg_b": gain(ks[16], (N_EVEN, SG_GROUPS, SG_CHUNK)),
        "mla_w_in": nrm(ks[17], (N_ODD, D, MLA_IN), D ** -0.5),
        "mla_q_norm_g": gain(ks[18], (N_ODD, Q_LORA)),
        "mla_kv_norm_g": gain(ks[19], (N_ODD, KV_LORA)),
        "mla_w_uq": nrm(ks[20], (N_ODD, Q_LORA, MLA_HEADS * QK_DIM), Q_LORA ** -0.5),
        "mla_w_ukv": nrm(ks[21], (N_ODD, KV_LORA, MLA_HEADS * (QK_NOPE + V_DIM)), KV_LORA ** -0.5),
        "mla_q_g": gain(ks[22], (N_ODD, QK_DIM)),
        "mla_k_g": gain(ks[23], (N_ODD, QK_DIM)),
        "mla_w_out": nrm(ks[24], (N_ODD, MLA_HEADS * V_DIM, D), (MLA_HEADS * V_DIM) ** -0.5),
        "moe_w_router": nrm(ks[25], (DEPTH, D, N_EXPERTS), D ** -0.5),
        "moe_b_router": nrm(ks[26], (DEPTH, N_EXPERTS), 0.01),
        "moe_w_gu": nrm(ks[27], (DEPTH, N_EXPERTS, D, 2 * D_FF), D ** -0.5),
        "moe_b_gu": nrm(ks[28], (DEPTH, N_EXPERTS, 2 * D_FF), 0.01),
        "moe_w_down": nrm(ks[29], (DEPTH, N_EXPERTS, D_FF, D), D_FF ** -0.5),
        "moe_b_down": nrm(ks[30], (DEPTH, N_EXPERTS, D), 0.01),
    }


def reference(x, c, ctx, c_ctx, ada_w, ada_b, norm1_g, norm2_g,
              ab_w_in, ab_w_out, na_q_g, na_k_g, na_rel_bias, sg_norm_g, sg_norm_b, sg_w, sg_b,
              mla_w_in, mla_q_norm_g, mla_kv_norm_g, mla_w_uq, mla_w_ukv, mla_q_g, mla_k_g, mla_w_out,
              moe_w_router, moe_b_router, moe_w_gu, moe_b_gu, moe_w_down, moe_b_down):
    B, S, D = x.shape
    T = ctx.shape[1]
    cos, sin = axial_rope_tables(S, QK_ROPE)
    xc = ctx
    for l in range(DEPTH):
        last = l == DEPTH - 1
        i = l // 2
        mod = jnp.split(jax.nn.silu(c) @ ada_w[l] + ada_b[l], 6, axis=-1)
        sh1, sc1, g1, sh2, sc2, g2 = [m[:, None, :] for m in mod]
        sh1c, sc1c, g1c, sh2c, sc2c, g2c = jnp.split(jax.nn.silu(c_ctx) @ ada_w[l] + ada_b[l], 6, axis=-1)
        h = modulate(rms_norm(x, norm1_g[l]), sh1, sc1)
        hc = modulate(rms_norm(xc, norm1_g[l]), sh1c, sc1c)
        if l % 2 == 0:
            y, yc = mixer_ab(h, hc, ab_w_in[i], ab_w_out[i], na_q_g[i], na_k_g[i], na_rel_bias[i],
                             sg_norm_g[i], sg_norm_b[i], sg_w[i], sg_b[i], not last)
        else:
            y, yc = mixer_mla(h, hc, cos, sin, mla_w_in[i], mla_q_norm_g[i], mla_kv_norm_g[i],
                              mla_w_uq[i], mla_w_ukv[i], mla_q_g[i], mla_k_g[i], mla_w_out[i], not last)
        x = x + g1 * y
        h2 = modulate(rms_norm(x, norm2_g[l]), sh2, sc2).reshape(B * S, D)
        if last:
            f = moe_ffn(h2, moe_w_router[l], moe_b_router[l], moe_w_gu[l], moe_b_gu[l],
                        moe_w_down[l], moe_b_down[l])
            x = x + g2 * f.reshape(B, S, D)
        else:
            xc = xc + g1c * yc
            h2c = modulate(rms_norm(xc, norm2_g[l]), sh2c, sc2c).reshape(B * T, D)
            f = moe_ffn(jnp.concatenate([h2c, h2], axis=0), moe_w_router[l], moe_b_router[l],
                        moe_w_gu[l], moe_b_gu[l], moe_w_down[l], moe_b_down[l])
            xc = xc + g2c * f[:B * T].reshape(B, T, D)
            x = x + g2 * f[B * T:].reshape(B, S, D)
    return x
```

```python
import numpy as np
from contextlib import ExitStack
import concourse.bass as bass
import concourse.mybir as mybir
from concourse.bass_utils import run_bass_kernel_spmd
import ml_dtypes

F32 = mybir.dt.float32
BF16 = mybir.dt.bfloat16
AF = mybir.ActivationFunctionType
ALU = mybir.AluOpType


class Buf:
    __slots__ = ("t", "w", "r", "name")

    def __init__(self, t, name=""):
        self.t = t
        self.w = {}
        self.r = {}
        self.name = name

    def __getitem__(self, idx):
        return self.t[idx]


class Prog:
    def __init__(self, nc, stack):
        self.nc = nc
        self.stack = stack
        self.root = stack
        self.engs = {"pe": nc.tensor, "act": nc.scalar, "dve": nc.vector,
                     "pool": nc.gpsimd, "sp": nc.sync}
        self.sem = {}
        self.cnt = {}
        self.waited = {k: {} for k in self.engs}
        for k in self.engs:
            self.sem[k] = stack.enter_context(nc.semaphore("s_" + k))
            self.cnt[k] = 0
        self.ninstr = 0

    def sbuf(self, name, shape, dt):
        self.uid = getattr(self, "uid", 0) + 1
        return Buf(self.stack.enter_context(self.nc.sbuf_tensor(f"{name}_{self.uid}", list(shape), dt)), name)

    def psum(self, name, shape, dt=F32):
        self.uid = getattr(self, "uid", 0) + 1
        return Buf(self.stack.enter_context(self.nc.psum_tensor(f"{name}_{self.uid}", list(shape), dt)), name)

    def dsem(self, key):
        if key not in self.sem:
            self.sem[key] = self.root.enter_context(self.nc.semaphore("d_" + key))
            self.cnt[key] = 0
        return key

    def _deps(self, reads, writes):
        deps = {}
        for b in reads:
            for k, v in b.w.items():
                if deps.get(k, 0) < v:
                    deps[k] = v
        for b in writes:
            for k, v in b.w.items():
                if deps.get(k, 0) < v:
                    deps[k] = v
            for k, v in b.r.items():
                if deps.get(k, 0) < v:
                    deps[k] = v
        return deps

    def _wait(self, e, deps):
        eng = self.engs[e]
        wd = self.waited[e]
        for k, v in deps.items():
            if e == "pe" and k == "pe":
                continue
            if wd.get(k, 0) < v:
                eng.wait_ge(self.sem[k], v)
                wd[k] = v

    def _mark(self, tok, reads, writes):
        k, v = tok
        for b in reads:
            b.r[k] = v
        for b in writes:
            b.w[k] = v
            b.r = {}

    def op(self, e, fn, reads=(), writes=()):
        self._wait(e, self._deps(reads, writes))
        ins = fn(self.engs[e])
        self.cnt[e] += 1
        ins.then_inc(self.sem[e], 1)
        self._mark((e, self.cnt[e]), reads, writes)
        self.ninstr += 1
        return ins

    def dma(self, q, key, out, in_, reads=(), writes=(), **kw):
        self.dsem(key)
        self._wait(q, self._deps(reads, writes))
        ins = self.engs[q].dma_start(out=out, in_=in_, **kw)
        self.cnt[key] += 16
        ins.then_inc(self.sem[key], 16)
        self._mark((key, self.cnt[key]), reads, writes)
        self.ninstr += 1
        return ins

    def finish(self, bufs):
        deps = {}
        for b in bufs:
            for k, v in b.w.items():
                deps[k] = max(deps.get(k, 0), v)
        self._wait("sp", deps)

    def barrier(self):
        deps = {k: v for k, v in self.cnt.items() if v > 0}
        for e in self.engs:
            self._wait(e, deps)

    def scope(self):
        return _Scope(self)


class _Scope:
    def __init__(self, P):
        self.P = P

    def __enter__(self):
        from contextlib import ExitStack
        self.old = self.P.stack
        self.st = ExitStack()
        self.st.__enter__()
        self.P.stack = self.st
        return self

    def __exit__(self, *a):
        self.P.barrier()
        self.P.stack = self.old
        return self.st.__exit__(*a)


U32 = mybir.dt.uint32
AX = mybir.AxisListType
NT = 2304
NTILE = 18
EPS = 1e-6


def norm_tmps(P, tag, head=False):
    T = {"sq": P.sbuf("sq" + tag, [128, 16, 512], BF16), "ss": P.psum("ss" + tag, [128, 512], F32),
         "rs": P.sbuf("rs" + tag, [128, 512], F32), "tmp": P.sbuf("tmp" + tag, [128, 512], F32)}
    if head:
        T.update({"hsq": P.sbuf("hsq" + tag, [128, 512], BF16), "hss": P.psum("hss" + tag, [128, 512], F32),
                  "hrs": P.sbuf("hrs" + tag, [128, 512], F32)})
    return T


def groups(nt):
    gs = [(0, 256)] if nt == 2304 else []
    t = 256 if nt == 2304 else 0
    while t < nt:
        gs.append((t, 512))
        t += 512
    return gs


def ada_phase(P, nc, adaw, adab_sb, cc_sb, MOD, ident=None):
    with P.scope():
        scc = P.sbuf("scc", [128, 16, 2], F32)
        P.op("act", lambda e: e.activation(out=scc[:], in_=cc_sb[:], func=AF.Silu), reads=[cc_sb], writes=[scc])
        wr = [P.sbuf(f"adaw{i}", [128, 16, 512], F32) for i in range(2)]
        pm = P.psum("pm", [128, 96, 2], F32)
        for pc in range(24):
            b = wr[pc % 2]
            P.dma("sp", f"adaw{pc % 2}", b[:], adaw[:, :, pc * 512:(pc + 1) * 512], writes=[b])
            for c4 in range(4):
                j = pc * 4 + c4
                for kc in range(16):
                    P.op("pe", lambda e: e.matmul(pm[:, j, :], lhsT=b[:, kc, c4 * 128:(c4 + 1) * 128], rhs=scc[:, kc, :],
                                                   start=(kc == 0), stop=(kc == 15)), reads=[b, scc], writes=[pm])
        for s in range(2):
            P.op("dve", lambda e: e.tensor_tensor(out=MOD[:, :, s], in0=pm[:, :, s], in1=adab_sb[:], op=ALU.add),
                 reads=[pm, adab_sb], writes=[MOD])


def norm_group(P, xg, TG, A, SH, shoff, s, outT, ones_bf, eps_sb, tag, T, out32=None):
    if True:
        sq, ss, rs, tmp = T["sq"], T["ss"], T["rs"], T["tmp"]
        P.op("act", lambda e: e.activation(out=sq[:, :, :TG], in_=xg[:, :, :TG], func=AF.Square), reads=[xg], writes=[sq])
        for dc in range(16):
            P.op("pe", lambda e: e.matmul(ss[:, :TG], lhsT=ones_bf[:], rhs=sq[:, dc, :TG], start=(dc == 0), stop=(dc == 15)),
                 reads=[ones_bf, sq], writes=[ss])
        P.op("act", lambda e: e.activation(out=rs[:, :TG], in_=ss[:, :TG], func=AF.Sqrt, bias=eps_sb[:, 0:1], scale=1.0 / 2048),
             reads=[ss, eps_sb], writes=[rs])
        P.op("dve", lambda e: e.reciprocal(out=rs[:, :TG], in_=rs[:, :TG]), reads=[rs], writes=[rs])
        for dc in range(16):
            P.op("dve", lambda e: e.scalar_tensor_tensor(out=tmp[:, :TG], in0=xg[:, dc, :TG], scalar=A[:, dc, s:s + 1], in1=rs[:, :TG],
                                                         op0=ALU.mult, op1=ALU.mult), reads=[xg, A, rs], writes=[tmp])
            if out32 is not None:
                P.op("act", lambda e: e.activation(out=out32[:, dc, :TG], in_=tmp[:, :TG], func=AF.Identity, bias=SH[:, shoff + dc, s:s + 1]),
                     reads=[tmp, SH], writes=[out32])
                P.op("pool", lambda e: e.tensor_copy(out=outT[:, dc, :TG], in_=out32[:, dc, :TG]), reads=[out32], writes=[outT])
            else:
                P.op("act", lambda e: e.activation(out=outT[:, dc, :TG], in_=tmp[:, :TG], func=AF.Identity, bias=SH[:, shoff + dc, s:s + 1]),
                     reads=[tmp, SH], writes=[outT])


def head_norm(P, ps, TG, gain_col, out_ap, out_buf, ones_bf, eps_sb, T):
    if True:
        sq, ss, rs = T["hsq"], T["hss"], T["hrs"]
        P.op("act", lambda e: e.activation(out=sq[:, :TG], in_=ps[:, :TG], func=AF.Square), reads=[ps], writes=[sq])
        P.op("pe", lambda e: e.matmul(ss[:, :TG], lhsT=ones_bf[:], rhs=sq[:, :TG], start=True, stop=True), reads=[ones_bf, sq], writes=[ss])
        P.op("act", lambda e: e.activation(out=rs[:, :TG], in_=ss[:, :TG], func=AF.Sqrt, bias=eps_sb[:, 0:1], scale=1.0 / 128),
             reads=[ss, eps_sb], writes=[rs])
        P.op("dve", lambda e: e.reciprocal(out=rs[:, :TG], in_=rs[:, :TG]), reads=[rs], writes=[rs])
        P.op("dve", lambda e: e.scalar_tensor_tensor(out=out_ap, in0=ps[:, :TG], scalar=gain_col, in1=rs[:, :TG], op0=ALU.mult, op1=ALU.mult),
             reads=[ps, rs], writes=[out_buf])


def gate_matrix(P, fz, lg, mx8, nm, ti):
    gm, gx, gp, ident = fz["gm"], fz["gx"], fz["gp"], fz["ident"]
    P.op("dve", lambda e: e.tensor_single_scalar(out=gm[:], in_=lg[:], scalar=mx8[:, 3:4], op=ALU.is_ge), reads=[lg, mx8], writes=[gm])
    P.op("act", lambda e: e.activation(out=gx[:], in_=lg[:], func=AF.Exp, bias=nm[:, 0:1]), reads=[lg, nm], writes=[gx])
    P.op("dve", lambda e: e.tensor_tensor(out=gx[:], in0=gx[:], in1=gm[:], op=ALU.mult), reads=[gx, gm], writes=[gx])
    P.op("dve", lambda e: e.tensor_scalar_mul(out=gx[:], in0=gx[:], scalar1=nm[:, 1:2]), reads=[gx, nm], writes=[gx])
    P.op("pe", lambda e: e.transpose(gp[:], gx[:], ident[:]), reads=[gx, ident], writes=[gp])
    P.op("act", lambda e: e.activation(out=fz["gt_sb"][:, ti * 128:(ti + 1) * 128], in_=gp[:], func=AF.Copy), reads=[gp], writes=[fz["gt_sb"]])


def build_k1(fz=None):
    pre = "" if fz is None else "a_"
    nc = bass.Bass("TRN2", target_bir_lowering=False) if fz is None else fz["nc"]
    D = lambda name, shape, dt=F32: nc.dram_tensor(pre + name, list(shape), dt, kind="ExternalInput").ap()
    O = (lambda name, shape, dt=F32: nc.dram_tensor(name, list(shape), dt, kind="ExternalOutput").ap()) if fz is None else \
        (lambda name, shape, dt=F32: nc.dram_tensor(pre + name, list(shape), dt).ap())
    I = lambda name, shape, dt=F32: Buf(nc.dram_tensor(pre + name, list(shape), dt).ap(), name)
    xT = D("xT", [128, 16, NT]); cc = D("cc", [128, 16, 2]); adaw = D("adaw", [128, 16, 12288]); adab = D("adab", [128, 96])
    n1g = D("n1g", [128, 16]); n2g = D("n2g", [128, 16])
    win = D("win", [128, 16, 5120]); wout = D("wout", [128, 16, 2048]); qkg = D("qkg", [128, 2])
    nab = D("nab", [8, 128, 3200]); nam = D("nam", [128, 3200])
    lng = D("lng", [128, 1024]); lnb = D("lnb", [128, 1024]); sgwT = D("sgwT", [128, 8, 128]); sgb = D("sgb", [1, 1024])
    wr = D("wr", [128, 16, 32]); br = D("br", [128, 32])
    X1T = Buf(O("X1T", [128, 16, NT]), "X1T"); H2T = Buf(O("H2T", [128, 16, NT], BF16), "H2T")
    IDX = Buf(O("IDX", [128, NTILE, 8], U32), "IDX"); GATE = Buf(O("GATE", [128, NTILE, 8]), "GATE")
    MODO = Buf(O("MODO", [128, 96, 2]), "MODO")
    QT = I("QT", [8, 128, NT], BF16); KT = I("KT", [8, 128, NT], BF16); V = I("V", [NT, 1024], BF16)
    CAT = I("CAT", [128, 16, NT], BF16)
    outs = [X1T, H2T, IDX, GATE, MODO]
    with (ExitStack() if fz is None else fz["P"].scope()) as st:
        P = Prog(nc, st) if fz is None else fz["P"]
        ones_bf = P.sbuf("ones_bf", [128, 128], BF16); P.op("dve", lambda e: e.memset(ones_bf[:], 1.0), writes=[ones_bf])
        eps_sb = P.sbuf("eps_sb", [128, 1], F32); P.op("dve", lambda e: e.memset(eps_sb[:], EPS), writes=[eps_sb])
        MOD = P.sbuf("MOD", [128, 96, 2], F32)
        small = {}
        for name, ap, shp in [("cc", cc, [128, 16, 2]), ("adab", adab, [128, 96]), ("n1g", n1g, [128, 16]), ("n2g", n2g, [128, 16]),
                              ("qkg", qkg, [128, 2])]:
            small[name] = P.sbuf("s_" + name, shp, F32)
            P.dma("sp", "ld_" + name, small[name][:], ap, writes=[small[name]])
        ada_phase(P, nc, adaw, small["adab"], small["cc"], MOD)
        P.dma("sp", "st_mod", MODO[:], MOD[:], reads=[MOD], writes=[MODO])
        A1 = P.sbuf("A1", [128, 16, 2], F32); A2 = P.sbuf("A2", [128, 16, 2], F32)
        for s in range(2):
            for (A, g, off) in ((A1, small["n1g"], 16), (A2, small["n2g"], 64)):
                P.op("dve", lambda e: e.scalar_tensor_tensor(out=A[:, :, s], in0=MOD[:, off:off + 16, s], scalar=1.0, in1=g[:], op0=ALU.add, op1=ALU.mult),
                     reads=[MOD, g], writes=[A])
        qkg_s = P.sbuf("qkg_s", [128, 2], F32)
        P.op("dve", lambda e: e.tensor_copy(out=qkg_s[:], in_=small["qkg"][:]), reads=[small["qkg"]], writes=[qkg_s])
        P.op("dve", lambda e: e.tensor_scalar_mul(out=qkg_s[:, 0:1], in0=small["qkg"][:, 0:1], scalar1=float(128 ** -0.5)),
             reads=[small["qkg"]], writes=[qkg_s])
        SH1 = lambda: (MOD, 0); G1 = 32; SH2 = 48; G2 = 80

        with P.scope():
            lng_sb = P.sbuf("lng_sb", [128, 1024], F32); lnb_sb = P.sbuf("lnb_sb", [128, 1024], F32)
            sgw_sb = P.sbuf("sgw_sb", [128, 8, 128], BF16); sgb_sb = P.sbuf("sgb_sb", [1, 1024], BF16)
            ones1 = P.sbuf("ones1", [1, 128], BF16); P.op("dve", lambda e: e.memset(ones1[:], 1.0), writes=[ones1])
            P.dma("sp", "ld_lng", lng_sb[:], lng, writes=[lng_sb]); P.dma("sp", "ld_lnb", lnb_sb[:], lnb, writes=[lnb_sb])
            P.dma("pool", "ld_sgw", sgw_sb[:], sgwT, writes=[sgw_sb]); P.dma("pool", "ld_sgb", sgb_sb[:], sgb, writes=[sgb_sb])
            xg = P.sbuf("xg", [128, 16, 512], F32)
            hT = P.sbuf("hT", [128, 16, 512], BF16)
            wp = [P.sbuf(f"wp{i}", [128, 16, 512], BF16) for i in range(3)]
            uT = P.sbuf("uT", [128, 8, 512], BF16)
            z = P.sbuf("z", [128, 4, 1024], F32)
            pfm = [P.psum(f"pfm{i}", [128, 512], F32) for i in range(2)]
            ptm = [P.psum(f"ptm{i}", [128, 512], F32) for i in range(2)]
            pmx = P.psum("pmx", [128, 8, 128], F32)
            stg = [P.sbuf(f"stg{i}", [128, 512], BF16) for i in range(2)]
            vst = [P.sbuf(f"vst{i}", [128, 512], BF16) for i in range(2)]
            zt = P.sbuf("zt", [128, 1024], F32); zsq = P.sbuf("zsq", [128, 1024], BF16); zln = P.sbuf("zln", [128, 1024], BF16)
            st4 = P.sbuf("st4", [128, 8], F32)
            gt = P.sbuf("gt", [128, 8, 128], BF16)
            T1 = norm_tmps(P, "p1", head=True)
            it = 0; ie = 0
            for (t0, TG) in groups(NT):
                s = 0 if t0 >= 256 else 1
                nsub = TG // 128
                P.dma("sp", "ld_xg", xg[:, :, :TG], xT[:, :, t0:t0 + TG], writes=[xg])
                norm_group(P, xg, TG, A1, MOD, 0, s, hT, ones_bf, eps_sb, "n1", T1)
                for pc in range(10):
                    if fz is not None:
                        fz["tick"]()
                    b = wp[it % 3]; key = f"ld_wp{it % 3}"; it += 1
                    for hf in range(2):
                        P.dma("pool", key, b[:, hf * 8:(hf + 1) * 8, :], win[:, hf * 8:(hf + 1) * 8, pc * 512:(pc + 1) * 512], writes=[b])
                    if pc < 4 or pc in (6, 7):
                        for c4 in range(4):
                            ps = pfm[ie % 2]; sg = stg[ie % 2]; ie += 1
                            for kc in range(16):
                                P.op("pe", lambda e: e.matmul(ps[:, :TG], lhsT=b[:, kc, c4 * 128:(c4 + 1) * 128], rhs=hT[:, kc, :TG],
                                                               start=(kc == 0), stop=(kc == 15)), reads=[b, hT], writes=[ps])
                            if pc < 4:
                                hd = (pc % 2) * 4 + c4
                                head_norm(P, ps, TG, qkg_s[:, (pc // 2):(pc // 2) + 1], sg[:, :TG], sg, ones_bf, eps_sb, T1)
                                dst = QT if pc < 2 else KT
                                P.dma("sp", f"st_qk{ie % 2}", dst.t[hd, :, t0:t0 + TG], sg[:, :TG], reads=[sg], writes=[dst])
                            else:
                                ch = (pc - 6) * 4 + c4
                                P.op("act", lambda e: e.activation(out=uT[:, ch, :TG], in_=ps[:, :TG], func=AF.Gelu_apprx_tanh), reads=[ps], writes=[uT])
                    else:
                        for sub in range(nsub):
                            ps = ptm[ie % 2]; vs = vst[ie % 2]; ie += 1
                            for kc in range(16):
                                P.op("pe", lambda e: e.matmul(ps[:], lhsT=hT[:, kc, sub * 128:(sub + 1) * 128], rhs=b[:, kc, :],
                                                               start=(kc == 0), stop=(kc == 15)), reads=[b, hT], writes=[ps])
                            if pc in (4, 5):
                                P.op("act", lambda e: e.activation(out=vs[:], in_=ps[:], func=AF.Copy), reads=[ps], writes=[vs])
                                r0 = t0 + sub * 128
                                P.dma("sp", f"st_v{ie % 2}", V.t[r0:r0 + 128, (pc - 4) * 512:(pc - 3) * 512], vs[:], reads=[vs], writes=[V])
                            else:
                                P.op("act", lambda e: e.activation(out=z[:, sub, (pc - 8) * 512:(pc - 7) * 512], in_=ps[:], func=AF.Gelu_apprx_tanh),
                                     reads=[ps], writes=[z])
                for sub in range(nsub):
                    zz = z[:, sub, :]
                    P.op("dve", lambda e: e.tensor_reduce(out=st4[:, 0:1], in_=zz, axis=AX.X, op=ALU.add), reads=[z], writes=[st4])
                    P.op("dve", lambda e: e.memset(st4[:, 1:2], 0.0), writes=[st4])
                    P.op("act", lambda e: e.activation(out=zsq[:], in_=zz, func=AF.Square, accum_out=st4[:, 1:2]), reads=[z], writes=[zsq, st4])
                    P.op("dve", lambda e: e.tensor_scalar_mul(out=st4[:, 2:3], in0=st4[:, 0:1], scalar1=1.0 / 1024), reads=[st4], writes=[st4])
                    P.op("dve", lambda e: e.tensor_tensor(out=st4[:, 3:4], in0=st4[:, 2:3], in1=st4[:, 2:3], op=ALU.mult), reads=[st4], writes=[st4])
                    P.op("dve", lambda e: e.scalar_tensor_tensor(out=st4[:, 4:5], in0=st4[:, 1:2], scalar=1.0 / 1024, in1=st4[:, 3:4], op0=ALU.mult, op1=ALU.subtract),
                         reads=[st4], writes=[st4])
                    P.op("act", lambda e: e.activation(out=st4[:, 5:6], in_=st4[:, 4:5], func=AF.Sqrt, bias=eps_sb[:, 0:1], scale=1.0), reads=[st4, eps_sb], writes=[st4])
                    P.op("dve", lambda e: e.reciprocal(out=st4[:, 6:7], in_=st4[:, 5:6]), reads=[st4], writes=[st4])
                    P.op("dve", lambda e: e.tensor_scalar(out=zt[:], in0=zz, scalar1=st4[:, 2:3], scalar2=st4[:, 6:7], op0=ALU.subtract, op1=ALU.mult),
                         reads=[z, st4], writes=[zt])
                    P.op("pool", lambda e: e.tensor_tensor(out=zt[:], in0=zt[:], in1=lng_sb[:], op=ALU.mult), reads=[zt, lng_sb], writes=[zt])
                    P.op("pool", lambda e: e.tensor_tensor(out=zln[:], in0=zt[:], in1=lnb_sb[:], op=ALU.add), reads=[zt, lnb_sb], writes=[zln])
                    for g in range(8):
                        P.op("pe", lambda e: e.matmul(pmx[:, g, :], lhsT=zln[:, g * 128:(g + 1) * 128], rhs=sgw_sb[:, g, :], start=True, stop=False),
                             reads=[zln, sgw_sb], writes=[pmx])
                        P.op("pe", lambda e: e.matmul(pmx[:, g, :], lhsT=ones1[:], rhs=sgb_sb[:, g * 128:(g + 1) * 128], start=False, stop=True),
                             reads=[ones1, sgb_sb], writes=[pmx])
                    P.op("dve", lambda e: e.tensor_tensor(out=gt[:], in0=uT[:, :, sub * 128:(sub + 1) * 128], in1=pmx[:], op=ALU.mult),
                         reads=[uT, pmx], writes=[gt])
                    r0 = t0 + sub * 128
                    P.dma("sp", "st_gt", CAT.t[:, 8:16, r0:r0 + 128], gt[:], reads=[gt], writes=[CAT])

        with P.scope():
            nam_sb = P.sbuf("nam_sb", [128, 3200], F32)
            P.dma("sp", "ld_nam", nam_sb[:], nam, writes=[nam_sb])
            bm = P.sbuf("bm", [128, 5, 5, 128], F32)
            qh = P.sbuf("qh", [128, NT], BF16); kh = P.sbuf("kh", [128, NT], BF16); vh = P.sbuf("vh", [128, NTILE, 128], BF16)
            ao = P.sbuf("ao", [128, NT], BF16)
            S = [P.psum(f"S{i}", [128, 8, 128], F32) for i in range(2)]
            Oo = [P.psum(f"Oo{i}", [128, 128], F32) for i in range(2)]
            Dn = [P.psum(f"Dn{i}", [128, 128], F32) for i in range(2)]
            sb = [P.sbuf(f"sb{i}", [128, 5, 128], F32) for i in range(2)]
            pt = [P.sbuf(f"pt{i}", [128, 7, 128], BF16) for i in range(2)]
            rc = [P.sbuf(f"rc{i}", [128, 128], F32) for i in range(2)]
            for h in range(8):
                if fz is not None:
                    fz["tick"]()
                P.dma("sp", "ld_bm", bm[:].rearrange("p a b c -> p (a b c)"), nab[h], writes=[bm])
                P.op("dve", lambda e: e.tensor_tensor(out=bm[:].rearrange("p a b c -> p (a b c)"), in0=bm[:].rearrange("p a b c -> p (a b c)"), in1=nam_sb[:], op=ALU.add),
                     reads=[bm, nam_sb], writes=[bm])
                P.dma("sp", "ld_qh", qh[:], QT.t[h], reads=[QT], writes=[qh])
                P.dma("sp", "ld_kh", kh[:], KT.t[h], reads=[KT], writes=[kh])
                P.dma("sp", "ld_vh", vh[:], V.t[:, h * 128:(h + 1) * 128].rearrange("(n p) d -> p n d", p=128), reads=[V], writes=[vh])
                for qi in range(NTILE):
                    Sx = S[qi % 2]; Ox = Oo[qi % 2]; Dx = Dn[qi % 2]; sbx = sb[qi % 2]; ptx = pt[qi % 2]; rcx = rc[qi % 2]
                    if qi < 2:
                        ktiles = [0, 1]; nloc = 0
                    else:
                        i = qi - 2
                        j0 = min(max(i - 2, 0), 11)
                        cls = 0 if i == 0 else 1 if i == 1 else 3 if i == 14 else 4 if i == 15 else 2
                        ktiles = [2 + j0 + a for a in range(5)] + [0, 1]; nloc = 5
                    nk = len(ktiles)
                    for a, kt in enumerate(ktiles):
                        P.op("pe", lambda e: e.matmul(Sx[:, a, :], lhsT=kh[:, kt * 128:(kt + 1) * 128], rhs=qh[:, qi * 128:(qi + 1) * 128], start=True, stop=True),
                             reads=[kh, qh], writes=[Sx])
                    if nloc:
                        P.op("dve", lambda e: e.tensor_tensor(out=sbx[:], in0=Sx[:, 0:5, :], in1=bm[:, cls, :, :], op=ALU.add), reads=[Sx, bm], writes=[sbx])
                        P.op("act", lambda e: e.activation(out=ptx[:, 0:5, :], in_=sbx[:], func=AF.Exp), reads=[sbx], writes=[ptx])
                    P.op("act", lambda e: e.activation(out=ptx[:, nloc:nk, :], in_=Sx[:, nloc:nk, :], func=AF.Exp), reads=[Sx], writes=[ptx])
                    for a, kt in enumerate(ktiles):
                        P.op("pe", lambda e: e.matmul(Ox[:], lhsT=vh[:, kt, :], rhs=ptx[:, a, :], start=(a == 0), stop=(a == nk - 1)), reads=[vh, ptx], writes=[Ox])
                    for a, kt in enumerate(ktiles):
                        P.op("pe", lambda e: e.matmul(Dx[:], lhsT=ones_bf[:], rhs=ptx[:, a, :], start=(a == 0), stop=(a == nk - 1)), reads=[ones_bf, ptx], writes=[Dx])
                    P.op("dve", lambda e: e.reciprocal(out=rcx[:], in_=Dx[:]), reads=[Dx], writes=[rcx])
                    P.op("dve", lambda e: e.tensor_tensor(out=ao[:, qi * 128:(qi + 1) * 128], in0=Ox[:], in1=rcx[:], op=ALU.mult), reads=[Ox, rcx], writes=[ao])
                P.dma("sp", "st_ao", CAT.t[:, h, :], ao[:], reads=[ao], writes=[CAT])

        with P.scope():
            wo = P.sbuf("wo", [128, 16, 2048], BF16)
            for q4 in range(4):
                P.dma("pool", "ld_wo", wo[:, q4 * 4:(q4 + 1) * 4, :], wout[:, q4 * 4:(q4 + 1) * 4, :], writes=[wo])
            wr_sb = P.sbuf("wr_sb", [128, 16, 32], F32); br_sb = P.sbuf("br_sb", [128, 32], F32)
            P.dma("sp", "ld_wr", wr_sb[:], wr, writes=[wr_sb]); P.dma("sp", "ld_br", br_sb[:], br, writes=[br_sb])
            xg = P.sbuf("xg3", [128, 16, 512], F32); cg = P.sbuf("cg3", [128, 16, 512], BF16)
            h2b = P.sbuf("h2b", [128, 16, 512], BF16); h2f = P.sbuf("h2f", [128, 16, 512], F32)
            py = [P.psum(f"py{i}", [128, 512], F32) for i in range(2)]
            pl = P.psum("pl", [128, 32], F32)
            if fz is not None:
                fz["gp"] = P.psum("gp", [32, 128], F32)
            lg = P.sbuf("lg", [128, 32], F32); mx8 = P.sbuf("mx8", [128, 8], F32)
            idx = P.sbuf("idx", [128, NTILE, 8], U32); gate = P.sbuf("gate", [128, NTILE, 8], F32)
            nm = P.sbuf("nm", [128, 2], F32)
            P.op("dve", lambda e: e.memset(gate[:], 0.0), writes=[gate])
            T3 = norm_tmps(P, "p3")
            for (t0, TG) in groups(NT):
                s = 0 if t0 >= 256 else 1
                P.dma("sp", "ld_xg3", xg[:, :, :TG], xT[:, :, t0:t0 + TG], writes=[xg])
                P.dma("sp", "ld_cg3", cg[:, :, :TG], CAT.t[:, :, t0:t0 + TG], reads=[CAT], writes=[cg])
                for dc in range(16):
                    ps = py[dc % 2]
                    for kc in range(16):
                        P.op("pe", lambda e: e.matmul(ps[:, :TG], lhsT=wo[:, kc, dc * 128:(dc + 1) * 128], rhs=cg[:, kc, :TG], start=(kc == 0), stop=(kc == 15)),
                             reads=[wo, cg], writes=[ps])
                    P.op("dve", lambda e: e.scalar_tensor_tensor(out=xg[:, dc, :TG], in0=ps[:, :TG], scalar=MOD[:, G1 + dc, s:s + 1], in1=xg[:, dc, :TG],
                                                                 op0=ALU.mult, op1=ALU.add), reads=[ps, MOD, xg], writes=[xg])
                P.dma("sp", "st_x1", X1T[:, :, t0:t0 + TG], xg[:, :, :TG], reads=[xg], writes=[X1T])
                norm_group(P, xg, TG, A2, MOD, SH2, s, h2b, ones_bf, eps_sb, "n2", T3, out32=h2f)
                P.dma("sp", "st_h2", H2T[:, :, t0:t0 + TG], h2b[:, :, :TG], reads=[h2b], writes=[H2T])
                for sub in range(TG // 128):
                    ti = (t0 + sub * 128) // 128
                    for kc in range(16):
                        P.op("pe", lambda e: e.matmul(pl[:], lhsT=h2f[:, kc, sub * 128:(sub + 1) * 128], rhs=wr_sb[:, kc, :], start=(kc == 0), stop=(kc == 15)),
                             reads=[h2f, wr_sb], writes=[pl])
                    P.op("dve", lambda e: e.tensor_tensor(out=lg[:], in0=pl[:], in1=br_sb[:], op=ALU.add), reads=[pl, br_sb], writes=[lg])
                    P.op("dve", lambda e: e.max(out=mx8[:], in_=lg[:]), reads=[lg], writes=[mx8])
                    P.op("dve", lambda e: e.max_index(out=idx[:, ti, :], in_max=mx8[:], in_values=lg[:]), reads=[mx8, lg], writes=[idx])
                    P.op("dve", lambda e: e.tensor_scalar_mul(out=nm[:, 0:1], in0=mx8[:, 0:1], scalar1=-1.0), reads=[mx8], writes=[nm])
                    P.op("dve", lambda e: e.memset(nm[:, 1:2], 0.0), writes=[nm])
                    P.op("act", lambda e: e.activation(out=gate[:, ti, 0:4], in_=mx8[:, 0:4], func=AF.Exp, bias=nm[:, 0:1], accum_out=nm[:, 1:2]),
                         reads=[mx8, nm], writes=[gate, nm])
                    P.op("dve", lambda e: e.reciprocal(out=nm[:, 1:2], in_=nm[:, 1:2]), reads=[nm], writes=[nm])
                    P.op("dve", lambda e: e.tensor_scalar_mul(out=gate[:, ti, 0:4], in0=gate[:, ti, 0:4], scalar1=nm[:, 1:2]),
                         reads=[gate, nm], writes=[gate])
                    if fz is not None:
                        gate_matrix(P, fz, lg, mx8, nm, ti)
            P.dma("sp", "st_idx", IDX[:], idx[:], reads=[idx], writes=[IDX])
            P.dma("sp", "st_gate", GATE[:], gate[:], reads=[gate], writes=[GATE])
            if fz is not None:
                P.dma("sp", "st_gt", fz["GT"].t[:, 0:NT], fz["gt_sb"][:, 0:NT], reads=[fz["gt_sb"]], writes=[fz["GT"]])
        if fz is None:
            P.finish(outs)
        print("K1 ninstr", P.ninstr)
    if fz is not None:
        return dict(X1T=X1T, H2T=H2T, MODO=MODO)
    return nc


def pkn(w):
    K, N = w.shape
    return np.ascontiguousarray(w.reshape(K // 128, 128, N).transpose(1, 0, 2))


def pvec(v, n=None):
    return np.ascontiguousarray(v.reshape(-1, 128).T)


def na_tables(rel_bias):
    H = rel_bias.shape[0]
    reps = [0, 1, 2, 14, 15]
    k = np.arange(128); q = np.arange(128)
    nab = np.zeros((H, 128, 5, 5, 128), np.float32); nam = np.zeros((128, 5, 5, 128), np.float32)
    for ci, i in enumerate(reps):
        j0 = min(max(i - 2, 0), 11)
        for a in range(5):
            j = j0 + a
            kr = (2 * j + k // 64)[:, None]; kc = (k % 64)[:, None]
            r = (2 * i + q // 64)[None, :]; qc = (q % 64)[None, :]
            r0 = np.clip(r - 4, 0, 24)
            cs = np.clip(qc - 8, 0, 48)
            valid = (kr >= r0) & (kr < r0 + 8) & (kc >= cs) & (kc < cs + 16)
            ri = np.clip(kr - r + 7, 0, 14); cidx = np.clip(kc - qc + 15, 0, 30)
            nab[:, :, ci, a, :] = rel_bias[:, ri, cidx]
            nam[:, ci, a, :] = np.where(valid, 0.0, -1e30)
    return nab.reshape(H, 128, 3200), nam.reshape(128, 3200)


def k1_inputs(inp, b):
    l = 0
    xcat = np.concatenate([inp["ctx"][b], inp["x"][b]], axis=0)
    nab, nam = na_tables(inp["na_rel_bias"][0])
    return {
        "xT": pkn(np.ascontiguousarray(xcat.T)),
        "cc": np.ascontiguousarray(np.stack([pvec(inp["c"][b]), pvec(inp["c_ctx"])], axis=-1)),
        "adaw": pkn(inp["ada_w"][l]), "adab": pvec(inp["ada_b"][l]),
        "n1g": pvec(inp["norm1_g"][l]), "n2g": pvec(inp["norm2_g"][l]),
        "win": pkn(inp["ab_w_in"][0]), "wout": pkn(inp["ab_w_out"][0]),
        "qkg": np.ascontiguousarray(np.stack([inp["na_q_g"][0], inp["na_k_g"][0]], axis=-1)),
        "nab": nab, "nam": nam,
        "lng": np.ascontiguousarray(np.broadcast_to(inp["sg_norm_g"][0], (128, 1024))),
        "lnb": np.ascontiguousarray(np.broadcast_to(inp["sg_norm_b"][0], (128, 1024))),
        "sgwT": np.ascontiguousarray(inp["sg_w"][0].transpose(2, 0, 1)),
        "sgb": np.ascontiguousarray(inp["sg_b"][0].reshape(1, 1024)),
        "wr": pkn(inp["moe_w_router"][l]), "br": np.ascontiguousarray(np.broadcast_to(inp["moe_b_router"][l], (128, 32))),
    }


CG = 2560
NE = 4


def build_k2(caps):
    nc = bass.Bass("TRN2", target_bir_lowering=False)
    D = lambda name, shape, dt=F32: nc.dram_tensor(name, list(shape), dt, kind="ExternalInput").ap()
    xs = [D(f"xs{j}", [128, 16, caps[j] * 512], BF16) for j in range(NE)]
    wgu = D("wgu", [NE, 128, 16, 4096]); bgu = D("bgu", [128, NE, 32])
    wd = D("wd", [NE, 128, 16, 2048]); bd = D("bd", [128, NE, 16])
    YS = [Buf(nc.dram_tensor(f"ys{j}", [128, 16, caps[j] * 512], F32, kind="ExternalOutput").ap(), f"ys{j}") for j in range(NE)]
    with ExitStack() as st:
        P = Prog(nc, st)
        bgu_sb = P.sbuf("bgu_sb", [128, NE, 32], F32); bd_sb = P.sbuf("bd_sb", [128, NE, 16], F32)
        P.dma("sp", "ld_bgu", bgu_sb[:], bgu, writes=[bgu_sb]); P.dma("sp", "ld_bd", bd_sb[:], bd, writes=[bd_sb])
        xg = [P.sbuf(f"xg{i}", [128, 16, 512], BF16) for i in range(2)]
        hid = P.sbuf("hid", [128, 16, 512], BF16)
        wp = [P.sbuf(f"wp{i}", [128, 16, 512], BF16) for i in range(4)]
        yo = [P.sbuf(f"yo{i}", [128, 4, 512], F32) for i in range(2)]
        pg = [P.psum(f"pg{i}", [128, 512], F32) for i in range(2)]
        pu = [P.psum(f"pu{i}", [128, 512], F32) for i in range(2)]
        py = [P.psum(f"py{i}", [128, 512], F32) for i in range(2)]
        tg = [P.sbuf(f"tg{i}", [128, 512], F32) for i in range(2)]
        ts = [P.sbuf(f"ts{i}", [128, 512], F32) for i in range(2)]
        tu = [P.sbuf(f"tu{i}", [128, 512], F32) for i in range(2)]
        iw = 0; ig = 0; io = 0

        def load_piece(src, e, c0):
            nonlocal iw
            b = wp[iw % 4]; key = f"ld_wp{iw % 4}"; iw += 1
            for hf in range(2):
                P.dma("pool", key, b[:, hf * 8:(hf + 1) * 8, :], src[e, :, hf * 8:(hf + 1) * 8, c0:c0 + 512], writes=[b])
            return b

        for e in range(NE):
            for sg in range(caps[e]):
                x = xg[ig % 2]; ig += 1
                P.dma("sp", f"ld_xg{ig % 2}", x[:], xs[e][:, :, sg * 512:(sg + 1) * 512], writes=[x])
                for f4 in range(4):
                    bg = load_piece(wgu, e, f4 * 512)
                    bu = load_piece(wgu, e, 2048 + f4 * 512)
                    for c4 in range(4):
                        fc = f4 * 4 + c4
                        k = fc % 2
                        for kc in range(16):
                            P.op("pe", lambda en: en.matmul(pg[k][:], lhsT=bg[:, kc, c4 * 128:(c4 + 1) * 128], rhs=x[:, kc, :], start=(kc == 0), stop=(kc == 15)),
                                 reads=[bg, x], writes=[pg[k]])
                        for kc in range(16):
                            P.op("pe", lambda en: en.matmul(pu[k][:], lhsT=bu[:, kc, c4 * 128:(c4 + 1) * 128], rhs=x[:, kc, :], start=(kc == 0), stop=(kc == 15)),
                                 reads=[bu, x], writes=[pu[k]])
                        P.op("dve", lambda en: en.tensor_scalar(out=tg[k][:], in0=pg[k][:], scalar1=bgu_sb[:, e, fc:fc + 1], scalar2=7.0, op0=ALU.add, op1=ALU.min),
                             reads=[pg[k], bgu_sb], writes=[tg[k]])
                        P.op("act", lambda en: en.activation(out=ts[k][:], in_=tg[k][:], func=AF.Sigmoid, scale=1.702), reads=[tg[k]], writes=[ts[k]])
                        P.op("dve", lambda en: en.tensor_scalar(out=tu[k][:], in0=pu[k][:], scalar1=bgu_sb[:, e, 16 + fc:16 + fc + 1], scalar2=7.0, op0=ALU.add, op1=ALU.min),
                             reads=[pu[k], bgu_sb], writes=[tu[k]])
                        P.op("pool", lambda en: en.tensor_scalar(out=tu[k][:], in0=tu[k][:], scalar1=-7.0, scalar2=1.0, op0=ALU.max, op1=ALU.add),
                             reads=[tu[k]], writes=[tu[k]])
                        P.op("pool", lambda en: en.tensor_tensor(out=tg[k][:], in0=tg[k][:], in1=ts[k][:], op=ALU.mult), reads=[tg[k], ts[k]], writes=[tg[k]])
                        P.op("dve", lambda en: en.tensor_tensor(out=hid[:, fc, :], in0=tg[k][:], in1=tu[k][:], op=ALU.mult), reads=[tg[k], tu[k]], writes=[hid])
                for d4 in range(4):
                    bw = load_piece(wd, e, d4 * 512)
                    y = yo[io % 2]; ykey = f"st_y{io % 2}"; io += 1
                    for c4 in range(4):
                        dc = d4 * 4 + c4
                        k = dc % 2
                        for fc in range(16):
                            P.op("pe", lambda en: en.matmul(py[k][:], lhsT=bw[:, fc, c4 * 128:(c4 + 1) * 128], rhs=hid[:, fc, :], start=(fc == 0), stop=(fc == 15)),
                                 reads=[bw, hid], writes=[py[k]])
                        P.op("act", lambda en: en.activation(out=y[:, c4, :], in_=py[k][:], func=AF.Identity, bias=bd_sb[:, e, dc:dc + 1]),
                             reads=[py[k], bd_sb], writes=[y])
                    P.dma("sp", ykey, YS[e][:, d4 * 4:(d4 + 1) * 4, sg * 512:(sg + 1) * 512], y[:], reads=[y], writes=[YS[e]])
        P.finish(YS)
        print("K2 ninstr", P.ninstr)
    return nc


def k2_weights(inp, l, es):
    return {
        "wgu": np.stack([pkn(inp["moe_w_gu"][l, e]) for e in es]),
        "bgu": np.ascontiguousarray(np.stack([inp["moe_b_gu"][l, e].reshape(32, 128).T for e in es], axis=1)),
        "wd": np.stack([pkn(inp["moe_w_down"][l, e]) for e in es]),
        "bd": np.ascontiguousarray(np.stack([inp["moe_b_down"][l, e].reshape(16, 128).T for e in es], axis=1)),
    }


NL = 2048


def combine_group(P, xg, yk, gb, g2buf, g2off, s, TG, tmpc):
    for dc in range(16):
        P.op("dve", lambda e: e.tensor_tensor(out=tmpc[0][:, :TG], in0=yk[0][:, dc, :TG], in1=gb[:, 0, :TG], op=ALU.mult), reads=[yk[0], gb], writes=[tmpc[0]])
        for k in range(1, 4):
            P.op("pool", lambda e: e.tensor_tensor(out=tmpc[1][:, :TG], in0=yk[k][:, dc, :TG], in1=gb[:, k, :TG], op=ALU.mult), reads=[yk[k], gb], writes=[tmpc[1]])
            P.op("dve", lambda e: e.tensor_tensor(out=tmpc[0][:, :TG], in0=tmpc[0][:, :TG], in1=tmpc[1][:, :TG], op=ALU.add), reads=[tmpc[0], tmpc[1]], writes=[tmpc[0]])
        P.op("dve", lambda e: e.scalar_tensor_tensor(out=xg[:, dc, :TG], in0=tmpc[0][:, :TG], scalar=g2buf[:, g2off + dc, s:s + 1], in1=xg[:, dc, :TG],
                                                     op0=ALU.mult, op1=ALU.add), reads=[tmpc[0], g2buf, xg], writes=[xg])


def build_k5():
    nc = bass.Bass("TRN2", target_bir_lowering=False)
    D = lambda name, shape, dt=F32: nc.dram_tensor(name, list(shape), dt, kind="ExternalInput").ap()
    xT = D("xT", [128, 16, NL]); y4 = D("y4", [4, 128, 16, NL]); gb_in = D("gb", [128, 4, NL]); modp = D("modp", [128, 96, 2])
    OUT = Buf(nc.dram_tensor("outT", [128, 16, NL], F32, kind="ExternalOutput").ap(), "outT")
    with ExitStack() as st:
        P = Prog(nc, st)
        MOD = P.sbuf("MOD", [128, 96, 2], F32); P.dma("sp", "ld_mod", MOD[:], modp, writes=[MOD])
        xg = P.sbuf("xg", [128, 16, 512], F32); yk = [P.sbuf(f"yk{k}", [128, 16, 512], F32) for k in range(4)]
        gb = P.sbuf("gbs", [128, 4, 512], F32); tmpc = [P.sbuf(f"tc{i}", [128, 512], F32) for i in range(2)]
        for g in range(4):
            t0 = g * 512
            P.dma("sp", "ld_xg", xg[:], xT[:, :, t0:t0 + 512], writes=[xg])
            P.dma("sp", "ld_gb", gb[:], gb_in[:, :, t0:t0 + 512], writes=[gb])
            for k in range(4):
                P.dma("sp", f"ld_yk{k}", yk[k][:], y4[k, :, :, t0:t0 + 512], writes=[yk[k]])
            combine_group(P, xg, yk, gb, MOD, 80, 0, 512, tmpc)
            P.dma("sp", "st_o", OUT[:, :, t0:t0 + 512], xg[:], reads=[xg], writes=[OUT])
        P.finish([OUT])
    return nc


def build_k4(fz=None):
    pre = "" if fz is None else "b_"
    nc = bass.Bass("TRN2", target_bir_lowering=False) if fz is None else fz["nc"]
    D = lambda name, shape, dt=F32: nc.dram_tensor(pre + name, list(shape), dt, kind="ExternalInput").ap()
    O = (lambda name, shape, dt=F32: nc.dram_tensor(name, list(shape), dt, kind="ExternalOutput").ap()) if fz is None else \
        (lambda name, shape, dt=F32: nc.dram_tensor(pre + name, list(shape), dt).ap())
    I = lambda name, shape, dt=F32: Buf(nc.dram_tensor(pre + name, list(shape), dt).ap(), name)
    if fz is None:
        xT = D("xT", [128, 16, NT]); y4 = D("y4", [4, 128, 16, NT]); gb_in = D("gb", [128, 4, NT]); modp = D("modp", [128, 96, 2])
    cc = D("cc", [128, 16, 2]); adaw = D("adaw", [128, 16, 12288]); adab = D("adab", [128, 96])
    n1g = D("n1g", [128, 16]); n2g = D("n2g", [128, 16])
    win = D("win", [128, 16, 832]); qng = D("qng", [128, 4]); kvng = D("kvng", [128, 2])
    wuq = D("wuq", [128, 4, 3072]); wukv = D("wukv", [128, 2, 4096])
    qg = D("qg", [128, 2]); kg = D("kg", [128, 2])
    cos2 = D("cos2", [64, NL]); sin2 = D("sin2", [64, NL]); pmT = D("pmT", [64, 64])
    wout = D("wout", [128, 16, 2048]); wr = D("wr", [128, 16, 32]); br = D("br", [128, 32])
    X3T = Buf(O("X3T", [128, 16, NL]), "X3T"); H2T = Buf(O("H2T", [128, 16, NL], BF16), "H2T")
    IDX = Buf(O("IDX", [128, 16, 8], U32), "IDX"); GATE = Buf(O("GATE", [128, 16, 8]), "GATE"); MODO = Buf(O("MODO", [128, 96, 2]), "MODO")
    X2 = I("X2", [128, 16, NT]) if fz is None else fz["X2"]
    QN = I("QN", [16, 128, NL], BF16); QR = I("QR", [16, 64, NL], BF16)
    KN = I("KN", [16, 128, NT], BF16); KR = I("KR", [16, 64, NT], BF16); V = I("V", [NT, 2048], BF16); CAT = I("CAT", [128, 16, NL], BF16)
    outs = [X3T, H2T, IDX, GATE, MODO]
    with (ExitStack() if fz is None else fz["P"].scope()) as st:
        P = Prog(nc, st) if fz is None else fz["P"]
        ones_bf = P.sbuf("ones_bf", [128, 128], BF16); P.op("dve", lambda e: e.memset(ones_bf[:], 1.0), writes=[ones_bf])
        eps_sb = P.sbuf("eps_sb", [128, 1], F32); P.op("dve", lambda e: e.memset(eps_sb[:], EPS), writes=[eps_sb])
        if fz is None:
            MOD0 = P.sbuf("MOD0", [128, 96, 2], F32); P.dma("sp", "ld_mod0", MOD0[:], modp, writes=[MOD0])
        MOD = P.sbuf("MOD", [128, 96, 2], F32)
        small = {}
        for name, ap, shp in [("cc", cc, [128, 16, 2]), ("adab", adab, [128, 96]), ("n1g", n1g, [128, 16]), ("n2g", n2g, [128, 16]),
                              ("qng", qng, [128, 4]), ("kvng", kvng, [128, 2]), ("qg", qg, [128, 2]), ("kg", kg, [128, 2])]:
            small[name] = P.sbuf("s_" + name, shp, F32)
            P.dma("sp", "ld_" + name, small[name][:], ap, writes=[small[name]])
        ada_phase(P, nc, adaw, small["adab"], small["cc"], MOD)
        P.dma("sp", "st_mod", MODO[:], MOD[:], reads=[MOD], writes=[MODO])
        A1 = P.sbuf("A1", [128, 16, 2], F32); A2 = P.sbuf("A2", [128, 16, 2], F32)
        for s in range(2):
            for (A, g, off) in ((A1, small["n1g"], 16), (A2, small["n2g"], 64)):
                P.op("dve", lambda e: e.scalar_tensor_tensor(out=A[:, :, s], in0=MOD[:, off:off + 16, s], scalar=1.0, in1=g[:], op0=ALU.add, op1=ALU.mult),
                     reads=[MOD, g], writes=[A])
        qg_s = P.sbuf("qg_s", [128, 2], F32)
        P.op("dve", lambda e: e.tensor_scalar_mul(out=qg_s[:], in0=small["qg"][:], scalar1=float(192 ** -0.5)), reads=[small["qg"]], writes=[qg_s])
        kg_s = small["kg"]
        G1 = 32; SH2 = 48

        with (P.scope() if fz is None else ExitStack()):
          if fz is None:
              xg = P.sbuf("xga", [128, 16, 512], F32); yk = [P.sbuf(f"yk{k}", [128, 16, 512], F32) for k in range(2)]
              gb = P.sbuf("gbs", [128, 4, 512], F32); tmpc = [P.sbuf(f"tc{i}", [128, 512], F32) for i in range(2)]
              for (t0, TG) in groups(NT):
                  s = 0 if t0 >= 256 else 1
                  P.dma("sp", "ld_xga", xg[:, :, :TG], xT[:, :, t0:t0 + TG], writes=[xg])
                  P.dma("sp", "ld_gb", gb[:, :, :TG], gb_in[:, :, t0:t0 + TG], writes=[gb])
                  for kk in range(2):
                      for k in range(2):
                          P.dma("sp", f"ld_yk{k}", yk[k][:, :, :TG], y4[kk * 2 + k, :, :, t0:t0 + TG], writes=[yk[k]])
                      for dc in range(16):
                          P.op("dve", lambda e: e.tensor_tensor(out=tmpc[0][:, :TG], in0=yk[0][:, dc, :TG], in1=gb[:, kk * 2, :TG], op=ALU.mult), reads=[yk[0], gb], writes=[tmpc[0]])
                          P.op("pool", lambda e: e.tensor_tensor(out=tmpc[1][:, :TG], in0=yk[1][:, dc, :TG], in1=gb[:, kk * 2 + 1, :TG], op=ALU.mult), reads=[yk[1], gb], writes=[tmpc[1]])
                          P.op("dve", lambda e: e.tensor_tensor(out=tmpc[0][:, :TG], in0=tmpc[0][:, :TG], in1=tmpc[1][:, :TG], op=ALU.add), reads=[tmpc[0], tmpc[1]], writes=[tmpc[0]])
                          P.op("dve", lambda e: e.scalar_tensor_tensor(out=xg[:, dc, :TG], in0=tmpc[0][:, :TG], scalar=MOD0[:, 80 + dc, s:s + 1], in1=xg[:, dc, :TG],
                                                                       op0=ALU.mult, op1=ALU.add), reads=[tmpc[0], MOD0, xg], writes=[xg])
                  P.dma("sp", "st_x2", X2.t[:, :, t0:t0 + TG], xg[:, :, :TG], reads=[xg], writes=[X2])

        with P.scope():
            win_sb = P.sbuf("win_sb", [128, 16, 832], BF16)
            for hf in range(2):
                P.dma("pool", "ld_win", win_sb[:, hf * 8:(hf + 1) * 8, :], win[:, hf * 8:(hf + 1) * 8, :], writes=[win_sb], max_dma_last_dim=1664)
            wuq_sb = P.sbuf("wuq_sb", [128, 4, 3072], BF16)
            for c in range(4):
                for hf in range(2):
                    P.dma("pool", "ld_wuq", wuq_sb[:, c, hf * 1536:(hf + 1) * 1536], wuq[:, c, hf * 1536:(hf + 1) * 1536], writes=[wuq_sb])
            wukv_sb = P.sbuf("wukv_sb", [128, 2, 4096], BF16)
            for c in range(2):
                for hf in range(2):
                    P.dma("pool", "ld_wukv", wukv_sb[:, c, hf * 2048:(hf + 1) * 2048], wukv[:, c, hf * 2048:(hf + 1) * 2048], writes=[wukv_sb])
            cos_sb = P.sbuf("cos_sb", [64, NL], F32); sin_sb = P.sbuf("sin_sb", [64, NL], F32); pm_sb = P.sbuf("pm_sb", [64, 64], BF16)
            P.dma("sp", "ld_cos", cos_sb[:], cos2, writes=[cos_sb]); P.dma("sp", "ld_sin", sin_sb[:], sin2, writes=[sin_sb])
            P.dma("pool", "ld_pm", pm_sb[:], pmT, writes=[pm_sb])
            xg = P.sbuf("xg", [128, 16, 512], F32)
            hT = P.sbuf("hT", [128, 16, 512], BF16)
            T1 = norm_tmps(P, "p1")
            c32 = P.sbuf("c32", [128, 4, 512], F32); csq = P.sbuf("csq", [128, 4, 512], BF16)
            cqT = P.sbuf("cqT", [128, 4, 512], BF16); ckvT = P.sbuf("ckvT", [128, 2, 512], BF16)
            kpe = P.sbuf("kpe", [64, 512], F32); kpg = P.sbuf("kpg", [64, 512], BF16); kpr = P.sbuf("kpr", [64, 512], F32)
            ksq = P.sbuf("ksq", [64, 512], BF16)
            pA = [P.psum(f"pA{i}", [128, 512], F32) for i in range(2)]
            pB = [P.psum(f"pB{i}", [64, 512], F32) for i in range(2)]
            pS2 = [P.psum(f"pS{i}", [128, 512], F32) for i in range(2)]; pS = pS2[0]
            pR = P.psum("pR", [64, 512], F32)
            rs2 = [P.sbuf(f"rs{i}", [128, 512], F32) for i in range(2)]; rs = rs2[0]
            sqn2 = [P.sbuf(f"sqn{i}", [128, 512], BF16) for i in range(2)]; sqn = sqn2[0]
            sqr2 = [P.sbuf(f"sqr{i}", [64, 512], BF16) for i in range(2)]; sqr = sqr2[0]
            qr_b = P.sbuf("qr_b", [64, 512], BF16); t1 = P.sbuf("t1", [64, 512], F32); t2 = P.sbuf("t2", [64, 512], F32)
            stn = [P.sbuf(f"stn{i}", [128, 512], BF16) for i in range(2)]
            str_ = [P.sbuf(f"str{i}", [64, 512], BF16) for i in range(2)]
            vst = [P.sbuf(f"vst{i}", [128, 128], BF16) for i in range(2)]
            io = 0

            def rstd_from(ss_ps, TG, scale):
                P.op("act", lambda e: e.activation(out=rs[:, :TG], in_=ss_ps[:, :TG], func=AF.Sqrt, bias=eps_sb[:, 0:1], scale=scale), reads=[ss_ps, eps_sb], writes=[rs])
                P.op("dve", lambda e: e.reciprocal(out=rs[:, :TG], in_=rs[:, :TG]), reads=[rs], writes=[rs])

            def lora_norm(c0, nch, gains, outT, TG):
                for c in range(nch):
                    ps = pA[c % 2]
                    for kc in range(16):
                        P.op("pe", lambda e: e.matmul(ps[:, :TG], lhsT=win_sb[:, kc, c0 + c * 128:c0 + (c + 1) * 128], rhs=hT[:, kc, :TG], start=(kc == 0), stop=(kc == 15)),
                             reads=[win_sb, hT], writes=[ps])
                    P.op("act", lambda e: e.activation(out=c32[:, c, :TG], in_=ps[:, :TG], func=AF.Copy), reads=[ps], writes=[c32])
                    P.op("act", lambda e: e.activation(out=csq[:, c, :TG], in_=ps[:, :TG], func=AF.Square), reads=[ps], writes=[csq])
                for c in range(nch):
                    P.op("pe", lambda e: e.matmul(pS[:, :TG], lhsT=ones_bf[:], rhs=csq[:, c, :TG], start=(c == 0), stop=(c == nch - 1)), reads=[ones_bf, csq], writes=[pS])
                rstd_from(pS, TG, 1.0 / (nch * 128))
                for c in range(nch):
                    P.op("dve", lambda e: e.scalar_tensor_tensor(out=outT[:, c, :TG], in0=c32[:, c, :TG], scalar=gains[:, c:c + 1], in1=rs[:, :TG], op0=ALU.mult, op1=ALU.mult),
                         reads=[c32, gains, rs], writes=[outT])

            def rope(src_b, src_buf, dst, dst_buf, TG, l0):
                P.op("pe", lambda e: e.matmul(pR[:, :TG], lhsT=pm_sb[:], rhs=src_b, start=True, stop=True), reads=[pm_sb, src_buf], writes=[pR])
                P.op("dve", lambda e: e.tensor_tensor(out=t1[:, :TG], in0=src_b, in1=cos_sb[:, l0:l0 + TG], op=ALU.mult), reads=[src_buf, cos_sb], writes=[t1])
                P.op("dve", lambda e: e.tensor_tensor(out=t2[:, :TG], in0=pR[:, :TG], in1=sin_sb[:, l0:l0 + TG], op=ALU.mult), reads=[pR, sin_sb], writes=[t2])
                P.op("dve", lambda e: e.tensor_tensor(out=dst, in0=t1[:, :TG], in1=t2[:, :TG], op=ALU.add), reads=[t1, t2], writes=[dst_buf])

            for (t0, TG) in [(tt, 256) for tt in range(0, NT, 256)]:
                s = 0 if t0 >= 256 else 1
                lat = t0 >= 256
                l0 = t0 - 256
                nsub = TG // 128
                P.dma("sp", "ld_xg", xg[:, :, :TG], X2.t[:, :, t0:t0 + TG], reads=[X2], writes=[xg])
                norm_group(P, xg, TG, A1, MOD, 0, s, hT, ones_bf, eps_sb, "n1", T1)
                lora_norm(512, 2, small["kvng"], ckvT, TG)
                for kc in range(16):
                    P.op("pe", lambda e: e.matmul(pB[0][:, :TG], lhsT=win_sb[:, kc, 768:832], rhs=hT[:, kc, :TG], start=(kc == 0), stop=(kc == 15)), reads=[win_sb, hT], writes=[pB[0]])
                P.op("act", lambda e: e.activation(out=kpe[:, :TG], in_=pB[0][:, :TG], func=AF.Copy), reads=[pB[0]], writes=[kpe])
                P.op("act", lambda e: e.activation(out=ksq[:, :TG], in_=pB[0][:, :TG], func=AF.Square), reads=[pB[0]], writes=[ksq])
                P.op("dve", lambda e: e.tensor_scalar_mul(out=kpg[:, :TG], in0=kpe[:, :TG], scalar1=kg_s[0:64, 1:2]), reads=[kpe, kg_s], writes=[kpg])
                if lat:
                    rope(kpg[:, :TG], kpg, kpr[:, :TG], kpr, TG, l0)
                else:
                    P.op("dve", lambda e: e.tensor_copy(out=kpr[:, :TG], in_=kpg[:, :TG]), reads=[kpg], writes=[kpr])
                for h in range(16):
                    pS, rs, sqn, sqr = pS2[h % 2], rs2[h % 2], sqn2[h % 2], sqr2[h % 2]
                    pn = pA[h % 2]
                    for c in range(2):
                        P.op("pe", lambda e: e.matmul(pn[:, :TG], lhsT=wukv_sb[:, c, h * 256:h * 256 + 128], rhs=ckvT[:, c, :TG], start=(c == 0), stop=(c == 1)), reads=[wukv_sb, ckvT], writes=[pn])
                    P.op("act", lambda e: e.activation(out=sqn[:, :TG], in_=pn[:, :TG], func=AF.Square), reads=[pn], writes=[sqn])
                    P.op("pe", lambda e: e.matmul(pS[:, :TG], lhsT=ones_bf[:], rhs=sqn[:, :TG], start=True, stop=False), reads=[ones_bf, sqn], writes=[pS])
                    P.op("pe", lambda e: e.matmul(pS[:, :TG], lhsT=ones_bf[0:64, :], rhs=ksq[:, :TG], start=False, stop=True), reads=[ones_bf, ksq], writes=[pS])
                    rstd_from(pS, TG, 1.0 / 192)
                    sn = stn[io % 2]; sr = str_[io % 2]; io += 1
                    P.op("dve", lambda e: e.scalar_tensor_tensor(out=sn[:, :TG], in0=pn[:, :TG], scalar=kg_s[:, 0:1], in1=rs[:, :TG], op0=ALU.mult, op1=ALU.mult), reads=[pn, kg_s, rs], writes=[sn])
                    P.op("pool", lambda e: e.tensor_tensor(out=sr[:, :TG], in0=kpr[:, :TG], in1=rs[0:64, :TG], op=ALU.mult), reads=[kpr, rs], writes=[sr])
                    P.dma("sp", f"st_kn{io % 2}", KN.t[h, :, t0:t0 + TG], sn[:, :TG], reads=[sn], writes=[KN])
                    P.dma("sp", f"st_kr{io % 2}", KR.t[h, :, t0:t0 + TG], sr[:, :TG], reads=[sr], writes=[KR])
                    for sub in range(nsub):
                        pv = pB[1]
                        vs = vst[(h * 4 + sub) % 2]
                        pvt = pA[(h + 1) % 2]
                        for c in range(2):
                            P.op("pe", lambda e: e.matmul(pvt[:, 0:128], lhsT=ckvT[:, c, sub * 128:(sub + 1) * 128], rhs=wukv_sb[:, c, h * 256 + 128:h * 256 + 256], start=(c == 0), stop=(c == 1)),
                                 reads=[ckvT, wukv_sb], writes=[pvt])
                        P.op("act", lambda e: e.activation(out=vs[:], in_=pvt[:, 0:128], func=AF.Copy), reads=[pvt], writes=[vs])
                        r0 = t0 + sub * 128
                        P.dma("sp", f"st_v{(h * 4 + sub) % 2}", V.t[r0:r0 + 128, h * 128:(h + 1) * 128], vs[:], reads=[vs], writes=[V])
                if not lat:
                    continue
                lora_norm(0, 4, small["qng"], cqT, TG)
                for h in range(16):
                    pS, rs, sqn, sqr = pS2[h % 2], rs2[h % 2], sqn2[h % 2], sqr2[h % 2]
                    pn = pA[h % 2]; pr = pB[h % 2]
                    for c in range(4):
                        P.op("pe", lambda e: e.matmul(pn[:, :TG], lhsT=wuq_sb[:, c, h * 192:h * 192 + 128], rhs=cqT[:, c, :TG], start=(c == 0), stop=(c == 3)), reads=[wuq_sb, cqT], writes=[pn])
                    for c in range(4):
                        P.op("pe", lambda e: e.matmul(pr[:, :TG], lhsT=wuq_sb[:, c, h * 192 + 128:h * 192 + 192], rhs=cqT[:, c, :TG], start=(c == 0), stop=(c == 3)), reads=[wuq_sb, cqT], writes=[pr])
                    P.op("act", lambda e: e.activation(out=sqn[:, :TG], in_=pn[:, :TG], func=AF.Square), reads=[pn], writes=[sqn])
                    P.op("act", lambda e: e.activation(out=sqr[:, :TG], in_=pr[:, :TG], func=AF.Square), reads=[pr], writes=[sqr])
                    P.op("pe", lambda e: e.matmul(pS[:, :TG], lhsT=ones_bf[:], rhs=sqn[:, :TG], start=True, stop=False), reads=[ones_bf, sqn], writes=[pS])
                    P.op("pe", lambda e: e.matmul(pS[:, :TG], lhsT=ones_bf[0:64, :], rhs=sqr[:, :TG], start=False, stop=True), reads=[ones_bf, sqr], writes=[pS])
                    rstd_from(pS, TG, 1.0 / 192)
                    sn = stn[io % 2]; sr = str_[io % 2]; io += 1
                    P.op("dve", lambda e: e.scalar_tensor_tensor(out=sn[:, :TG], in0=pn[:, :TG], scalar=qg_s[:, 0:1], in1=rs[:, :TG], op0=ALU.mult, op1=ALU.mult), reads=[pn, qg_s, rs], writes=[sn])
                    P.op("dve", lambda e: e.scalar_tensor_tensor(out=qr_b[:, :TG], in0=pr[:, :TG], scalar=qg_s[0:64, 1:2], in1=rs[0:64, :TG], op0=ALU.mult, op1=ALU.mult), reads=[pr, qg_s, rs], writes=[qr_b])
                    rope(qr_b[:, :TG], qr_b, sr[:, :TG], sr, TG, l0)
                    P.dma("sp", f"st_kn{io % 2}", QN.t[h, :, l0:l0 + TG], sn[:, :TG], reads=[sn], writes=[QN])
                    P.dma("sp", f"st_kr{io % 2}", QR.t[h, :, l0:l0 + TG], sr[:, :TG], reads=[sr], writes=[QR])

        with P.scope():
            qn = P.sbuf("qn", [128, NL], BF16); qr = P.sbuf("qr", [64, NL], BF16)
            kn = P.sbuf("kn", [128, NT], BF16); kr = P.sbuf("kr", [64, NT], BF16)
            vh = P.sbuf("vh", [128, 18, 128], BF16); ao = P.sbuf("ao", [128, NL], BF16)
            S = [P.psum(f"S{i}", [128, 512], F32) for i in range(2)]
            Oo = P.psum("Oo", [128, 512], F32); Dn = P.psum("Dn", [128, 512], F32)
            pt = [P.sbuf(f"pt{i}", [128, 512], BF16) for i in range(2)]
            rc = P.sbuf("rc", [128, 512], F32)
            for h in range(16):
                P.dma("sp", "ld_qn", qn[:], QN.t[h], reads=[QN], writes=[qn]); P.dma("sp", "ld_qr", qr[:], QR.t[h], reads=[QR], writes=[qr])
                P.dma("sp", "ld_kn", kn[:], KN.t[h], reads=[KN], writes=[kn]); P.dma("sp", "ld_kr", kr[:], KR.t[h], reads=[KR], writes=[kr])
                P.dma("sp", "ld_vh", vh[:], V.t[:, h * 128:(h + 1) * 128].rearrange("(n p) d -> p n d", p=128), reads=[V], writes=[vh])
                for qg4 in range(4):
                    q0 = qg4 * 512
                    for kt in range(18):
                        Sx = S[kt % 2]; px = pt[kt % 2]
                        P.op("pe", lambda e: e.matmul(Sx[:], lhsT=kn[:, kt * 128:(kt + 1) * 128], rhs=qn[:, q0:q0 + 512], start=True, stop=False), reads=[kn, qn], writes=[Sx])
                        P.op("pe", lambda e: e.matmul(Sx[:], lhsT=kr[:, kt * 128:(kt + 1) * 128], rhs=qr[:, q0:q0 + 512], start=False, stop=True), reads=[kr, qr], writes=[Sx])
                        P.op("act", lambda e: e.activation(out=px[:], in_=Sx[:], func=AF.Exp), reads=[Sx], writes=[px])
                        P.op("pe", lambda e: e.matmul(Oo[:], lhsT=vh[:, kt, :], rhs=px[:], start=(kt == 0), stop=(kt == 17)), reads=[vh, px], writes=[Oo])
                        P.op("pe", lambda e: e.matmul(Dn[:], lhsT=ones_bf[:], rhs=px[:], start=(kt == 0), stop=(kt == 17)), reads=[ones_bf, px], writes=[Dn])
                    P.op("dve", lambda e: e.reciprocal(out=rc[:], in_=Dn[:]), reads=[Dn], writes=[rc])
                    P.op("dve", lambda e: e.tensor_tensor(out=ao[:, q0:q0 + 512], in0=Oo[:], in1=rc[:], op=ALU.mult), reads=[Oo, rc], writes=[ao])
                P.dma("sp", "st_ao", CAT.t[:, h, :], ao[:], reads=[ao], writes=[CAT])

        with P.scope():
            wo = P.sbuf("wo", [128, 16, 2048], BF16)
            for q4 in range(4):
                P.dma("pool", "ld_wo", wo[:, q4 * 4:(q4 + 1) * 4, :], wout[:, q4 * 4:(q4 + 1) * 4, :], writes=[wo])
            wr_sb = P.sbuf("wr_sb", [128, 16, 32], F32); br_sb = P.sbuf("br_sb", [128, 32], F32)
            P.dma("sp", "ld_wr", wr_sb[:], wr, writes=[wr_sb]); P.dma("sp", "ld_br", br_sb[:], br, writes=[br_sb])
            xg = P.sbuf("xg3", [128, 16, 512], F32); cg = P.sbuf("cg3", [128, 16, 512], BF16)
            h2b = P.sbuf("h2b", [128, 16, 512], BF16); h2f = P.sbuf("h2f", [128, 16, 512], F32)
            py = [P.psum(f"py{i}", [128, 512], F32) for i in range(2)]
            pl = P.psum("pl", [128, 32], F32)
            if fz is not None:
                fz["gp"] = P.psum("gp", [32, 128], F32)
            lg = P.sbuf("lg", [128, 32], F32); mx8 = P.sbuf("mx8", [128, 8], F32)
            idx = P.sbuf("idx", [128, 16, 8], U32); gate = P.sbuf("gate", [128, 16, 8], F32)
            nm = P.sbuf("nm", [128, 2], F32)
            P.op("dve", lambda e: e.memset(gate[:], 0.0), writes=[gate])
            T3 = norm_tmps(P, "p3")
            for g4 in range(4):
                t0 = g4 * 512; TG = 512; s = 0
                P.dma("sp", "ld_xg3", xg[:], X2.t[:, :, 256 + t0:256 + t0 + TG], reads=[X2], writes=[xg])
                P.dma("sp", "ld_cg3", cg[:], CAT.t[:, :, t0:t0 + TG], reads=[CAT], writes=[cg])
                for dc in range(16):
                    ps = py[dc % 2]
                    for kc in range(16):
                        P.op("pe", lambda e: e.matmul(ps[:], lhsT=wo[:, kc, dc * 128:(dc + 1) * 128], rhs=cg[:, kc, :], start=(kc == 0), stop=(kc == 15)), reads=[wo, cg], writes=[ps])
                    P.op("dve", lambda e: e.scalar_tensor_tensor(out=xg[:, dc, :], in0=ps[:], scalar=MOD[:, G1 + dc, 0:1], in1=xg[:, dc, :], op0=ALU.mult, op1=ALU.add), reads=[ps, MOD, xg], writes=[xg])
                P.dma("sp", "st_x3", X3T[:, :, t0:t0 + TG], xg[:], reads=[xg], writes=[X3T])
                norm_group(P, xg, TG, A2, MOD, SH2, 0, h2b, ones_bf, eps_sb, "n2", T3, out32=h2f)
                P.dma("sp", "st_h2", H2T[:, :, t0:t0 + TG], h2b[:], reads=[h2b], writes=[H2T])
                for sub in range(4):
                    ti = g4 * 4 + sub
                    for kc in range(16):
                        P.op("pe", lambda e: e.matmul(pl[:], lhsT=h2f[:, kc, sub * 128:(sub + 1) * 128], rhs=wr_sb[:, kc, :], start=(kc == 0), stop=(kc == 15)), reads=[h2f, wr_sb], writes=[pl])
                    P.op("dve", lambda e: e.tensor_tensor(out=lg[:], in0=pl[:], in1=br_sb[:], op=ALU.add), reads=[pl, br_sb], writes=[lg])
                    P.op("dve", lambda e: e.max(out=mx8[:], in_=lg[:]), reads=[lg], writes=[mx8])
                    P.op("dve", lambda e: e.max_index(out=idx[:, ti, :], in_max=mx8[:], in_values=lg[:]), reads=[mx8, lg], writes=[idx])
                    P.op("dve", lambda e: e.tensor_scalar_mul(out=nm[:, 0:1], in0=mx8[:, 0:1], scalar1=-1.0), reads=[mx8], writes=[nm])
                    P.op("dve", lambda e: e.memset(nm[:, 1:2], 0.0), writes=[nm])
                    P.op("act", lambda e: e.activation(out=gate[:, ti, 0:4], in_=mx8[:, 0:4], func=AF.Exp, bias=nm[:, 0:1], accum_out=nm[:, 1:2]), reads=[mx8, nm], writes=[gate, nm])
                    P.op("dve", lambda e: e.reciprocal(out=nm[:, 1:2], in_=nm[:, 1:2]), reads=[nm], writes=[nm])
                    P.op("dve", lambda e: e.tensor_scalar_mul(out=gate[:, ti, 0:4], in0=gate[:, ti, 0:4], scalar1=nm[:, 1:2]), reads=[gate, nm], writes=[gate])
                    if fz is not None:
                        gate_matrix(P, fz, lg, mx8, nm, ti)
            P.dma("sp", "st_idx", IDX[:], idx[:], reads=[idx], writes=[IDX])
            P.dma("sp", "st_gate", GATE[:], gate[:], reads=[gate], writes=[GATE])
            if fz is not None:
                P.dma("sp", "st_gt", fz["GT"].t[:, 0:NL], fz["gt_sb"][:, 0:NL], reads=[fz["gt_sb"]], writes=[fz["GT"]])
        if fz is None:
            P.finish(outs)
        print("K4 ninstr", P.ninstr)
    if fz is not None:
        return dict(X3T=X3T, H2T=H2T, MODO=MODO)
    return nc


def cast_jobs(P, wsrc, W16, ncols, key):
    jobs = []
    for e in range(32):
        W = W16[e // 8]
        for kc in range(16):
            for c0 in range(0, ncols, 2048):
                pc0 = c0 // 512
                jobs.append(lambda W=W, e=e, kc=kc, c0=c0, pc0=pc0: P.dma(
                    "pool", key, W.t[e % 8, pc0:pc0 + 4, :, kc, :].rearrange("pc p n -> p pc n"),
                    wsrc[e, kc * 128:(kc + 1) * 128, c0:c0 + 2048].rearrange("p (pc n) -> p pc n", n=512), writes=[W]))
    return jobs


def moe_dense(P, nc, l, grp, Xin, H2, GT, Xout, MODO, WGU16, WD16, bgu, bd, sel):
    with P.scope():
        mod = P.sbuf("mmod", [128, 96, 2], F32); P.dma("sp", "ld_mmod", mod[:], MODO[:], reads=[MODO], writes=[mod])
        bgu_sb = P.sbuf("mbgu", [128, 32, 32], F32); bd_sb = P.sbuf("mbd", [128, 32, 16], F32)
        P.dma("sp", "ld_mbgu", bgu_sb[:], bgu[:, l], writes=[bgu_sb]); P.dma("sp", "ld_mbd", bd_sb[:], bd[:, l], writes=[bd_sb])
        sel_sb = P.sbuf("msel", [32, 32, 128], F32); P.dma("sp", "ld_msel", sel_sb[:], sel, writes=[sel_sb])
        gtg = P.sbuf("mgtg", [32, 512], F32)
        xg = P.sbuf("mxg", [128, 16, 512], F32); hg = P.sbuf("mhg", [128, 16, 512], BF16); hid = P.sbuf("mhid", [128, 16, 512], BF16)
        wp = [P.sbuf(f"mwp{i}", [128, 16, 512], BF16) for i in range(4)]
        gbs = P.sbuf("mgbs", [128, 512], F32)
        pg = [P.psum(f"mpg{i}", [128, 512], F32) for i in range(2)]
        pu = [P.psum(f"mpu{i}", [128, 512], F32) for i in range(2)]
        py = [P.psum(f"mpy{i}", [128, 512], F32) for i in range(2)]
        pgb = P.psum("mpgb", [128, 512], F32)
        tg = [P.sbuf(f"mtg{i}", [128, 512], F32) for i in range(2)]
        ts = [P.sbuf(f"mts{i}", [128, 512], F32) for i in range(2)]
        tu = [P.sbuf(f"mtu{i}", [128, 512], F32) for i in range(2)]
        ty = [P.sbuf(f"mty{i}", [128, 512], F32) for i in range(2)]
        iw = 0

        def load_piece(W, e, c0):
            nonlocal iw
            b = wp[iw % 4]; key = f"ld_mwp{iw % 4}"; iw += 1
            Wb = W[e // 8]
            P.dma("sp", key, b[:], Wb.t[e % 8, c0 // 512], reads=[Wb], writes=[b])
            return b

        for (t0, TG) in grp:
            s = 1 if (Xin.t.shape[2] == 2304 and t0 < 256) else 0
            P.dma("sp", "ld_mxg", xg[:, :, :TG], Xin.t[:, :, t0:t0 + TG], reads=[Xin], writes=[xg])
            P.dma("sp", "ld_mhg", hg[:, :, :TG], H2.t[:, :, t0:t0 + TG], reads=[H2], writes=[hg])
            P.dma("sp", "ld_mgt", gtg[:, :TG], GT.t[:, t0:t0 + TG], reads=[GT], writes=[gtg])
            for e in range(32):
                P.op("pe", lambda en: en.matmul(pgb[:, :TG], lhsT=sel_sb[:, e, :], rhs=gtg[:, :TG], start=True, stop=True), reads=[sel_sb, gtg], writes=[pgb])
                P.op("act", lambda en: en.activation(out=gbs[:, :TG], in_=pgb[:, :TG], func=AF.Copy), reads=[pgb], writes=[gbs])
                for f4 in range(4):
                    bg = load_piece(WGU16, e, f4 * 512)
                    bu = load_piece(WGU16, e, 2048 + f4 * 512)
                    for c4 in range(4):
                        fc = f4 * 4 + c4
                        k = fc % 2
                        for kc in range(16):
                            P.op("pe", lambda en: en.matmul(pg[k][:, :TG], lhsT=bg[:, kc, c4 * 128:(c4 + 1) * 128], rhs=hg[:, kc, :TG], start=(kc == 0), stop=(kc == 15)),
                                 reads=[bg, hg], writes=[pg[k]])
                        for kc in range(16):
                            P.op("pe", lambda en: en.matmul(pu[k][:, :TG], lhsT=bu[:, kc, c4 * 128:(c4 + 1) * 128], rhs=hg[:, kc, :TG], start=(kc == 0), stop=(kc == 15)),
                                 reads=[bu, hg], writes=[pu[k]])
                        P.op("dve", lambda en: en.tensor_scalar(out=tg[k][:, :TG], in0=pg[k][:, :TG], scalar1=bgu_sb[:, e, fc:fc + 1], scalar2=7.0, op0=ALU.add, op1=ALU.min),
                             reads=[pg[k], bgu_sb], writes=[tg[k]])
                        P.op("act", lambda en: en.activation(out=ts[k][:, :TG], in_=tg[k][:, :TG], func=AF.Sigmoid, scale=1.702), reads=[tg[k]], writes=[ts[k]])
                        P.op("dve", lambda en: en.tensor_scalar(out=tu[k][:, :TG], in0=pu[k][:, :TG], scalar1=bgu_sb[:, e, 16 + fc:16 + fc + 1], scalar2=7.0, op0=ALU.add, op1=ALU.min),
                             reads=[pu[k], bgu_sb], writes=[tu[k]])
                        P.op("dve", lambda en: en.tensor_scalar(out=tu[k][:, :TG], in0=tu[k][:, :TG], scalar1=-7.0, scalar2=1.0, op0=ALU.max, op1=ALU.add),
                             reads=[tu[k]], writes=[tu[k]])
                        P.op("dve", lambda en: en.tensor_tensor(out=tg[k][:, :TG], in0=tg[k][:, :TG], in1=ts[k][:, :TG], op=ALU.mult), reads=[tg[k], ts[k]], writes=[tg[k]])
                        P.op("dve", lambda en: en.tensor_tensor(out=hid[:, fc, :TG], in0=tg[k][:, :TG], in1=tu[k][:, :TG], op=ALU.mult), reads=[tg[k], tu[k]], writes=[hid])
                for d4 in range(4):
                    bw = load_piece(WD16, e, d4 * 512)
                    for c4 in range(4):
                        dc = d4 * 4 + c4
                        k = dc % 2
                        for fc in range(16):
                            P.op("pe", lambda en: en.matmul(py[k][:, :TG], lhsT=bw[:, fc, c4 * 128:(c4 + 1) * 128], rhs=hid[:, fc, :TG], start=(fc == 0), stop=(fc == 15)),
                                 reads=[bw, hid], writes=[py[k]])
                        P.op("dve", lambda en: en.scalar_tensor_tensor(out=ty[k][:, :TG], in0=py[k][:, :TG], scalar=bd_sb[:, e, dc:dc + 1], in1=gbs[:, :TG], op0=ALU.add, op1=ALU.mult),
                             reads=[py[k], bd_sb, gbs], writes=[ty[k]])
                        P.op("dve", lambda en: en.scalar_tensor_tensor(out=xg[:, dc, :TG], in0=ty[k][:, :TG], scalar=mod[:, 80 + dc, s:s + 1], in1=xg[:, dc, :TG], op0=ALU.mult, op1=ALU.add),
                             reads=[ty[k], mod, xg], writes=[xg])
            P.dma("sp", "st_mx", Xout.t[:, :, t0:t0 + TG], xg[:, :, :TG], reads=[xg], writes=[Xout])


def build_fused():
    nc = bass.Bass("TRN2", target_bir_lowering=False)
    D = lambda name, shape, dt=F32: nc.dram_tensor(name, list(shape), dt, kind="ExternalInput").ap()
    I = lambda name, shape, dt=F32: Buf(nc.dram_tensor(name, list(shape), dt).ap(), name)
    wgu = [D(f"wgu{l}", [32, 2048, 4096]) for l in range(2)]; wd = [D(f"wd{l}", [32, 2048, 2048]) for l in range(2)]
    bgu = D("mbgu_in", [128, 2, 32, 32]); bd = D("mbd_in", [128, 2, 32, 16]); sel = D("msel_in", [32, 32, 128]); ident_in = D("ident_in", [128, 128])
    OUT = Buf(nc.dram_tensor("outT", [128, 16, NL], F32, kind="ExternalOutput").ap(), "outT")
    WGU16 = [[I(f"WGU16_{l}_{q}", [8, 8, 128, 16, 512], BF16) for q in range(4)] for l in range(2)]
    WD16 = [[I(f"WD16_{l}_{q}", [8, 4, 128, 16, 512], BF16) for q in range(4)] for l in range(2)]
    GT = [I("GT0", [32, NT]), I("GT1", [32, NL])]; X2 = I("X2f", [128, 16, NT])
    with ExitStack() as st:
        P = Prog(nc, st)
        ident = P.sbuf("ident", [128, 128], F32); P.dma("sp", "ld_ident", ident[:], ident_in, writes=[ident])
        fz = {"nc": nc, "P": P, "ident": ident, "gm": P.sbuf("gm", [128, 32], F32), "gx": P.sbuf("gx", [128, 32], F32),
              "gt_sb": P.sbuf("gt_sb", [32, NT], F32), "GT": GT[0], "X2": X2}
        jobs0 = cast_jobs(P, wgu[0], WGU16[0], 4096, "cast0") + cast_jobs(P, wd[0], WD16[0], 2048, "cast0")
        NTICK = 60
        per = -(-len(jobs0) // NTICK)

        def tick():
            for _ in range(per):
                if jobs0:
                    jobs0.pop(0)()
        fz["tick"] = tick
        r1 = build_k1(fz)
        while jobs0:
            jobs0.pop(0)()
        fz["tick"] = lambda: None
        for j in cast_jobs(P, wgu[1], WGU16[1], 4096, "cast1") + cast_jobs(P, wd[1], WD16[1], 2048, "cast1"):
            j()
        moe_dense(P, nc, 0, groups(NT), r1["X1T"], r1["H2T"], GT[0], X2, r1["MODO"], WGU16[0], WD16[0], bgu, bd, sel)
        fz["GT"] = GT[1]
        r4 = build_k4(fz)
        moe_dense(P, nc, 1, [(t, 512) for t in range(0, NL, 512)], r4["X3T"], r4["H2T"], GT[1], OUT, r4["MODO"], WGU16[1], WD16[1], bgu, bd, sel)
        P.finish([OUT])
        print("FUSED ninstr", P.ninstr)
    return nc


def fused_inputs(inp, b):
    d = {"a_" + k: v for k, v in k1_inputs(inp, b).items()}
    cos2, sin2, pmT = _rope_consts()
    l = 1
    d.update({
        "b_cc": d["a_cc"], "b_adaw": pkn(inp["ada_w"][l]), "b_adab": pvec(inp["ada_b"][l]), "b_n1g": pvec(inp["norm1_g"][l]), "b_n2g": pvec(inp["norm2_g"][l]),
        "b_win": pkn(inp["mla_w_in"][0]), "b_qng": pvec(inp["mla_q_norm_g"][0]), "b_kvng": pvec(inp["mla_kv_norm_g"][0]),
        "b_wuq": pkn(inp["mla_w_uq"][0]), "b_wukv": pkn(inp["mla_w_ukv"][0]),
        "b_qg": _pad_gain(inp["mla_q_g"][0]), "b_kg": _pad_gain(inp["mla_k_g"][0]),
        "b_cos2": cos2, "b_sin2": sin2, "b_pmT": pmT, "b_wout": pkn(inp["mla_w_out"][0]),
        "b_wr": pkn(inp["moe_w_router"][l]), "b_br": np.ascontiguousarray(np.broadcast_to(inp["moe_b_router"][l], (128, 32))),
        "wgu0": inp["moe_w_gu"][0], "wgu1": inp["moe_w_gu"][1], "wd0": inp["moe_w_down"][0], "wd1": inp["moe_w_down"][1],
        "mbgu_in": np.ascontiguousarray(inp["moe_b_gu"].reshape(2, 32, 32, 128).transpose(3, 0, 1, 2)),
        "mbd_in": np.ascontiguousarray(inp["moe_b_down"].reshape(2, 32, 16, 128).transpose(3, 0, 1, 2)),
        "msel_in": np.ascontiguousarray(np.broadcast_to(np.eye(32, dtype=np.float32)[:, :, None], (32, 32, 128))),
        "ident_in": np.eye(128, dtype=np.float32),
    })
    return d


def kernel_fused_1core(inp, b):
    nc = build_fused()
    res = run_bass_kernel_spmd(nc, [fused_inputs(inp, b)], core_ids=[0])
    return _tok_major(np.asarray(res.results[0]["outT"]))


NCORES = 8


def _run(nc, in_maps):
    res = run_bass_kernel_spmd(nc, in_maps, core_ids=list(range(NCORES)))
    return res.results


def _tok_major(a):
    return np.ascontiguousarray(a.transpose(2, 1, 0).reshape(a.shape[2], 2048))


def _feat_major(a):
    T = a.shape[-2]
    lead = a.shape[:-2]
    b = a.reshape(*lead, T, 16, 128)
    n = len(lead)
    return np.ascontiguousarray(b.transpose(*range(n), n + 2, n + 1, n))


def _moe_route_and_run(inp, l, res, T):
    h2 = [_tok_major(np.asarray(r["H2T"])) for r in res]
    idx = [np.asarray(r["IDX"]).transpose(1, 0, 2).reshape(T, 8)[:, :4].astype(np.int64) for r in res]
    gate = [np.asarray(r["GATE"]).transpose(1, 0, 2).reshape(T, 8)[:, :4] for r in res]
    e_flat = np.concatenate([i.reshape(-1) for i in idx])
    order = np.argsort(e_flat, kind="stable")
    counts = np.bincount(e_flat, minlength=32)
    starts = np.cumsum(counts) - counts
    pos = np.empty_like(e_flat)
    pos[order] = np.arange(e_flat.size) - starts[e_flat[order]]
    ng = np.maximum(1, -(-counts // 512))
    rank = np.argsort(-ng, kind="stable")
    caps = [int(ng[rank[8 * j]]) for j in range(NE)]
    nc2 = build_k2(caps)
    h2_all = np.concatenate(h2, axis=0)
    tok_of = np.arange(e_flat.size) // 4
    ins = []
    for c in range(NCORES):
        es = [int(rank[8 * j + c]) for j in range(NE)]
        d = k2_weights(inp, l, es)
        for j, e in enumerate(es):
            xs = np.zeros((caps[j] * 512, 2048), dtype=h2_all.dtype)
            sel = np.nonzero(e_flat == e)[0]
            xs[pos[sel]] = h2_all[tok_of[sel]]
            d[f"xs{j}"] = _feat_major(xs)
        ins.append(d)
    r2 = _run(nc2, ins)
    y_assign = np.zeros((e_flat.size, 2048), np.float32)
    for c in range(NCORES):
        for j in range(NE):
            e = int(rank[8 * j + c])
            ys = _tok_major(np.asarray(r2[c][f"ys{j}"]))
            sel = np.nonzero(e_flat == e)[0]
            y_assign[sel] = ys[pos[sel]]
    y_assign = y_assign.reshape(NCORES, T, 4, 2048)
    out = []
    for b in range(NCORES):
        y4 = _feat_major(np.ascontiguousarray(y_assign[b].transpose(1, 0, 2)))
        gbc = np.ascontiguousarray(np.broadcast_to(gate[b].T[None], (128, 4, T))).astype(np.float32)
        out.append((y4, gbc))
    return out


def _rope_consts():
    t = np.arange(2048)
    row = (t // 64).astype(np.float32); col = (t % 64).astype(np.float32)
    inv = (np.float32(10000.0) ** (-np.arange(16, dtype=np.float32) / np.float32(16))).astype(np.float32)
    ang = np.concatenate([row[:, None] * inv, col[:, None] * inv], axis=-1).astype(np.float32)
    cos = np.cos(ang).astype(np.float32); sin = np.sin(ang).astype(np.float32)
    cos2 = np.ascontiguousarray(np.concatenate([cos.T, cos.T], axis=0)); sin2 = np.ascontiguousarray(np.concatenate([sin.T, sin.T], axis=0))
    pmT = np.zeros((64, 64), np.float32)
    for m in range(32):
        pmT[m + 32, m] = -1.0
    for m in range(32, 64):
        pmT[m - 32, m] = 1.0
    return cos2, sin2, pmT


def _pad_gain(g):
    out = np.zeros((128, 2), np.float32)
    out[:, 0] = g[:128]
    out[:64, 1] = g[128:192]
    return out


def kernel_unfused(**inp):
    inp = {k: np.asarray(v) for k, v in inp.items()}
    nc1 = build_k1()
    r1 = _run(nc1, [k1_inputs(inp, b) for b in range(NCORES)])
    m0 = _moe_route_and_run(inp, 0, r1, 2304)
    cos2, sin2, pmT = _rope_consts()
    l = 1
    shared = {
        "adaw": pkn(inp["ada_w"][l]), "adab": pvec(inp["ada_b"][l]), "n1g": pvec(inp["norm1_g"][l]), "n2g": pvec(inp["norm2_g"][l]),
        "win": pkn(inp["mla_w_in"][0]), "qng": pvec(inp["mla_q_norm_g"][0]), "kvng": pvec(inp["mla_kv_norm_g"][0]),
        "wuq": pkn(inp["mla_w_uq"][0]), "wukv": pkn(inp["mla_w_ukv"][0]),
        "qg": _pad_gain(inp["mla_q_g"][0]), "kg": _pad_gain(inp["mla_k_g"][0]),
        "cos2": cos2, "sin2": sin2, "pmT": pmT, "wout": pkn(inp["mla_w_out"][0]),
        "wr": pkn(inp["moe_w_router"][l]), "br": np.ascontiguousarray(np.broadcast_to(inp["moe_b_router"][l], (128, 32))),
    }
    ins4 = []
    for b in range(NCORES):
        d = dict(shared)
        d["xT"] = np.asarray(r1[b]["X1T"]); d["y4"] = m0[b][0]; d["gb"] = m0[b][1]; d["modp"] = np.asarray(r1[b]["MODO"])
        d["cc"] = np.ascontiguousarray(np.stack([pvec(inp["c"][b]), pvec(inp["c_ctx"])], axis=-1))
        ins4.append(d)
    nc4 = build_k4()
    r4 = _run(nc4, ins4)
    del m0, ins4
    m1 = _moe_route_and_run(inp, 1, r4, 2048)
    nc5 = build_k5()
    ins5 = [{"xT": np.asarray(r4[b]["X3T"]), "y4": m1[b][0], "gb": m1[b][1], "modp": np.asarray(r4[b]["MODO"])} for b in range(NCORES)]
    r5 = _run(nc5, ins5)
    out = np.stack([_tok_major(np.asarray(r5[b]["outT"])) for b in range(NCORES)], axis=0).astype(np.float32)
    return out


def kernel(**inp):
    inp = {k: np.asarray(v) for k, v in inp.items()}
    nc = build_fused()
    shared = fused_inputs(inp, 0)
    ins = []
    for b in range(NCORES):
        d = dict(shared)
        xcat = np.concatenate([inp["ctx"][b], inp["x"][b]], axis=0)
        d["a_xT"] = pkn(np.ascontiguousarray(xcat.T))
        cc = np.ascontiguousarray(np.stack([pvec(inp["c"][b]), pvec(inp["c_ctx"])], axis=-1))
        d["a_cc"] = cc; d["b_cc"] = cc
        ins.append(d)
    res = _run(nc, ins)
    return np.stack([_tok_major(np.asarray(res[b]["outT"])) for b in range(NCORES)], axis=0).astype(np.float32)
```

```python
import numpy as np
from contextlib import ExitStack
import concourse.bass as bass
import concourse.mybir as mybir
from concourse.bass_utils import run_bass_kernel_spmd
import ml_dtypes

F32 = mybir.dt.float32
BF16 = mybir.dt.bfloat16
AF = mybir.ActivationFunctionType
ALU = mybir.AluOpType


class Buf:
    __slots__ = ("t", "w", "r", "name")

    def __init__(self, t, name=""):
        self.t = t
        self.w = {}
        self.r = {}
        self.name = name

    def __getitem__(self, idx):
        return self.t[idx]


class Prog:
    def __init__(self, nc, stack):
        self.nc = nc
        self.stack = stack
        self.root = stack
        self.engs = {"pe": nc.tensor, "act": nc.scalar, "dve": nc.vector,
                     "pool": nc.gpsimd, "sp": nc.sync}
        self.sem = {}
        self.cnt = {}
        self.waited = {k: {} for k in self.engs}
        for k in self.engs:
            self.sem[k] = stack.enter_context(nc.semaphore("s_" + k))
            self.cnt[k] = 0
        self.ninstr = 0

    def sbuf(self, name, shape, dt):
        self.uid = getattr(self, "uid", 0) + 1
        return Buf(self.stack.enter_context(self.nc.sbuf_tensor(f"{name}_{self.uid}", list(shape), dt)), name)

    def psum(self, name, shape, dt=F32):
        self.uid = getattr(self, "uid", 0) + 1
        return Buf(self.stack.enter_context(self.nc.psum_tensor(f"{name}_{self.uid}", list(shape), dt)), name)

    def dsem(self, key):
        if key not in self.sem:
            self.sem[key] = self.root.enter_context(self.nc.semaphore("d_" + key))
            self.cnt[key] = 0
        return key

    def _deps(self, reads, writes):
        deps = {}
        for b in reads:
            for k, v in b.w.items():
                if deps.get(k, 0) < v:
                    deps[k] = v
        for b in writes:
            for k, v in b.w.items():
                if deps.get(k, 0) < v:
                    deps[k] = v
            for k, v in b.r.items():
                if deps.get(k, 0) < v:
                    deps[k] = v
        return deps

    def _wait(self, e, deps):
        eng = self.engs[e]
        wd = self.waited[e]
        for k, v in deps.items():
            if e == "pe" and k == "pe":
                continue
            if wd.get(k, 0) < v:
                eng.wait_ge(self.sem[k], v)
                wd[k] = v

    def _mark(self, tok, reads, writes):
        k, v = tok
        for b in reads:
            b.r[k] = v
        for b in writes:
            b.w[k] = v
            b.r = {}

    def op(self, e, fn, reads=(), writes=(), signal=True):
        self._wait(e, self._deps(reads, writes))
        ins = fn(self.engs[e])
        self.ninstr += 1
        pend = self.__dict__.setdefault("pend", {}).setdefault(e, ([], []))
        if not signal:
            pend[0].extend(reads); pend[1].extend(writes)
            return ins
        self.cnt[e] += 1
        ins.then_inc(self.sem[e], 1)
        self._mark((e, self.cnt[e]), list(reads) + pend[0], list(writes) + pend[1])
        pend[0].clear(); pend[1].clear()
        return ins

    def dma(self, q, key, out, in_, reads=(), writes=(), **kw):
        self.dsem(key)
        self._wait(q, self._deps(reads, writes))
        ins = self.engs[q].dma_start(out=out, in_=in_, **kw)
        self.cnt[key] += 16
        ins.then_inc(self.sem[key], 16)
        self._mark((key, self.cnt[key]), reads, writes)
        self.ninstr += 1
        return ins

    def finish(self, bufs):
        deps = {}
        for b in bufs:
            for k, v in b.w.items():
                deps[k] = max(deps.get(k, 0), v)
        self._wait("sp", deps)

    def barrier(self):
        deps = {k: v for k, v in self.cnt.items() if v > 0}
        for e in self.engs:
            self._wait(e, deps)

    def scope(self):
        return _Scope(self)


class _Scope:
    def __init__(self, P):
        self.P = P

    def __enter__(self):
        from contextlib import ExitStack
        self.old = self.P.stack
        self.st = ExitStack()
        self.st.__enter__()
        self.P.stack = self.st
        return self

    def __exit__(self, *a):
        self.P.barrier()
        self.P.stack = self.old
        return self.st.__exit__(*a)


U32 = mybir.dt.uint32
AX = mybir.AxisListType
NT = 2304
NTILE = 18
EPS = 1e-6


def norm_tmps(P, tag, head=False):
    T = {"sq": P.sbuf("sq" + tag, [128, 16, 512], BF16), "ss": P.psum("ss" + tag, [128, 512], F32),
         "rs": P.sbuf("rs" + tag, [128, 512], F32), "tmp": P.sbuf("tmp" + tag, [128, 512], F32)}
    if head:
        T.update({"hsq": P.sbuf("hsq" + tag, [128, 512], BF16), "hss": P.psum("hss" + tag, [128, 512], F32),
                  "hrs": P.sbuf("hrs" + tag, [128, 512], F32)})
    return T


def groups(nt):
    gs = [(0, 256)] if nt == 2304 else []
    t = 256 if nt == 2304 else 0
    while t < nt:
        gs.append((t, 512))
        t += 512
    return gs


def ada_phase(P, nc, adaw, adab_sb, cc_sb, MOD, ident=None):
    with P.scope():
        scc = P.sbuf("scc", [128, 16, 2], F32)
        P.op("act", lambda e: e.activation(out=scc[:], in_=cc_sb[:], func=AF.Silu), reads=[cc_sb], writes=[scc])
        wr = [P.sbuf(f"adaw{i}", [128, 16, 512], F32) for i in range(2)]
        pm = P.psum("pm", [128, 96, 2], F32)
        for pc in range(24):
            b = wr[pc % 2]
            P.dma("sp", f"adaw{pc % 2}", b[:], adaw[:, :, pc * 512:(pc + 1) * 512], writes=[b])
            for c4 in range(4):
                j = pc * 4 + c4
                for kc in range(16):
                    P.op("pe", lambda e: e.matmul(pm[:, j, :], lhsT=b[:, kc, c4 * 128:(c4 + 1) * 128], rhs=scc[:, kc, :],
                                                   start=(kc == 0), stop=(kc == 15)), reads=[b, scc], writes=[pm])
        for s in range(2):
            P.op("dve", lambda e: e.tensor_tensor(out=MOD[:, :, s], in0=pm[:, :, s], in1=adab_sb[:], op=ALU.add),
                 reads=[pm, adab_sb], writes=[MOD])


def norm_group(P, xg, TG, A, SH, shoff, s, outT, ones_bf, eps_sb, tag, T, out32=None):
    if True:
        sq, ss, rs, tmp = T["sq"], T["ss"], T["rs"], T["tmp"]
        P.op("act", lambda e: e.activation(out=sq[:, :, :TG], in_=xg[:, :, :TG], func=AF.Square), reads=[xg], writes=[sq])
        for dc in range(16):
            P.op("pe", lambda e: e.matmul(ss[:, :TG], lhsT=ones_bf[:], rhs=sq[:, dc, :TG], start=(dc == 0), stop=(dc == 15)),
                 reads=[ones_bf, sq], writes=[ss])
        P.op("act", lambda e: e.activation(out=rs[:, :TG], in_=ss[:, :TG], func=AF.Sqrt, bias=eps_sb[:, 0:1], scale=1.0 / 2048),
             reads=[ss, eps_sb], writes=[rs])
        P.op("dve", lambda e: e.reciprocal(out=rs[:, :TG], in_=rs[:, :TG]), reads=[rs], writes=[rs])
        for dc in range(16):
            P.op("dve", lambda e: e.scalar_tensor_tensor(out=tmp[:, :TG], in0=xg[:, dc, :TG], scalar=A[:, dc, s:s + 1], in1=rs[:, :TG],
                                                         op0=ALU.mult, op1=ALU.mult), reads=[xg, A, rs], writes=[tmp])
            if out32 is not None:
                P.op("act", lambda e: e.activation(out=out32[:, dc, :TG], in_=tmp[:, :TG], func=AF.Identity, bias=SH[:, shoff + dc, s:s + 1]),
                     reads=[tmp, SH], writes=[out32])
                P.op("pool", lambda e: e.tensor_copy(out=outT[:, dc, :TG], in_=out32[:, dc, :TG]), reads=[out32], writes=[outT])
            else:
                P.op("act", lambda e: e.activation(out=outT[:, dc, :TG], in_=tmp[:, :TG], func=AF.Identity, bias=SH[:, shoff + dc, s:s + 1]),
                     reads=[tmp, SH], writes=[outT])


def head_norm(P, ps, TG, gain_col, out_ap, out_buf, ones_bf, eps_sb, T):
    if True:
        sq, ss, rs = T["hsq"], T["hss"], T["hrs"]
        P.op("act", lambda e: e.activation(out=sq[:, :TG], in_=ps[:, :TG], func=AF.Square), reads=[ps], writes=[sq])
        P.op("pe", lambda e: e.matmul(ss[:, :TG], lhsT=ones_bf[:], rhs=sq[:, :TG], start=True, stop=True), reads=[ones_bf, sq], writes=[ss])
        P.op("act", lambda e: e.activation(out=rs[:, :TG], in_=ss[:, :TG], func=AF.Sqrt, bias=eps_sb[:, 0:1], scale=1.0 / 128),
             reads=[ss, eps_sb], writes=[rs])
        P.op("dve", lambda e: e.reciprocal(out=rs[:, :TG], in_=rs[:, :TG]), reads=[rs], writes=[rs])
        P.op("dve", lambda e: e.scalar_tensor_tensor(out=out_ap, in0=ps[:, :TG], scalar=gain_col, in1=rs[:, :TG], op0=ALU.mult, op1=ALU.mult),
             reads=[ps, rs], writes=[out_buf])


def gate_matrix(P, fz, lg, mx8, nm, ti):
    gm, gx, gp, ident = fz["gm"], fz["gx"], fz["gp"], fz["ident"]
    P.op("dve", lambda e: e.tensor_single_scalar(out=gm[:], in_=lg[:], scalar=mx8[:, 3:4], op=ALU.is_ge), reads=[lg, mx8], writes=[gm])
    P.op("act", lambda e: e.activation(out=gx[:], in_=lg[:], func=AF.Exp, bias=nm[:, 0:1]), reads=[lg, nm], writes=[gx])
    P.op("dve", lambda e: e.tensor_tensor(out=gx[:], in0=gx[:], in1=gm[:], op=ALU.mult), reads=[gx, gm], writes=[gx])
    P.op("dve", lambda e: e.tensor_scalar_mul(out=gx[:], in0=gx[:], scalar1=nm[:, 1:2]), reads=[gx, nm], writes=[gx])
    P.op("pe", lambda e: e.transpose(gp[:], gx[:], ident[:]), reads=[gx, ident], writes=[gp])
    P.op("act", lambda e: e.activation(out=fz["gt_sb"][:, ti * 128:(ti + 1) * 128], in_=gp[:], func=AF.Copy), reads=[gp], writes=[fz["gt_sb"]])


def build_k1(fz=None):
    pre = "" if fz is None else "a_"
    nc = bass.Bass("TRN2", target_bir_lowering=False) if fz is None else fz["nc"]
    D = lambda name, shape, dt=F32: nc.dram_tensor(pre + name, list(shape), dt, kind="ExternalInput").ap()
    O = (lambda name, shape, dt=F32: nc.dram_tensor(name, list(shape), dt, kind="ExternalOutput").ap()) if fz is None else \
        (lambda name, shape, dt=F32: nc.dram_tensor(pre + name, list(shape), dt).ap())
    I = lambda name, shape, dt=F32: Buf(nc.dram_tensor(pre + name, list(shape), dt).ap(), name)
    xT = D("xT", [128, 16, NT]); cc = D("cc", [128, 16, 2]); adaw = D("adaw", [128, 16, 12288]); adab = D("adab", [128, 96])
    n1g = D("n1g", [128, 16]); n2g = D("n2g", [128, 16])
    win = D("win", [128, 16, 5120]); wout = D("wout", [128, 16, 2048]); qkg = D("qkg", [128, 2])
    nab = D("nab", [8, 128, 3200]); nam = D("nam", [128, 3200])
    lng = D("lng", [128, 1024]); lnb = D("lnb", [128, 1024]); sgwT = D("sgwT", [128, 8, 128]); sgb = D("sgb", [1, 1024])
    wr = D("wr", [128, 16, 32]); br = D("br", [128, 32])
    X1T = Buf(O("X1T", [128, 16, NT]), "X1T"); H2T = Buf(O("H2T", [128, 16, NT], BF16), "H2T")
    IDX = Buf(O("IDX", [128, NTILE, 8], U32), "IDX"); GATE = Buf(O("GATE", [128, NTILE, 8]), "GATE")
    MODO = Buf(O("MODO", [128, 96, 2]), "MODO")
    QT = I("QT", [8, 128, NT], BF16); KT = I("KT", [8, 128, NT], BF16); V = I("V", [NT, 1024], BF16)
    CAT = I("CAT", [128, 16, NT], BF16)
    outs = [X1T, H2T, IDX, GATE, MODO]
    with (ExitStack() if fz is None else fz["P"].scope()) as st:
        P = Prog(nc, st) if fz is None else fz["P"]
        ones_bf = P.sbuf("ones_bf", [128, 128], BF16); P.op("dve", lambda e: e.memset(ones_bf[:], 1.0), writes=[ones_bf])
        eps_sb = P.sbuf("eps_sb", [128, 1], F32); P.op("dve", lambda e: e.memset(eps_sb[:], EPS), writes=[eps_sb])
        MOD = P.sbuf("MOD", [128, 96, 2], F32)
        small = {}
        for name, ap, shp in [("cc", cc, [128, 16, 2]), ("adab", adab, [128, 96]), ("n1g", n1g, [128, 16]), ("n2g", n2g, [128, 16]),
                              ("qkg", qkg, [128, 2])]:
            small[name] = P.sbuf("s_" + name, shp, F32)
            P.dma("sp", "ld_" + name, small[name][:], ap, writes=[small[name]])
        ada_phase(P, nc, adaw, small["adab"], small["cc"], MOD)
        P.dma("sp", "st_mod", MODO[:], MOD[:], reads=[MOD], writes=[MODO])
        A1 = P.sbuf("A1", [128, 16, 2], F32); A2 = P.sbuf("A2", [128, 16, 2], F32)
        for s in range(2):
            for (A, g, off) in ((A1, small["n1g"], 16), (A2, small["n2g"], 64)):
                P.op("dve", lambda e: e.scalar_tensor_tensor(out=A[:, :, s], in0=MOD[:, off:off + 16, s], scalar=1.0, in1=g[:], op0=ALU.add, op1=ALU.mult),
                     reads=[MOD, g], writes=[A])
        qkg_s = P.sbuf("qkg_s", [128, 2], F32)
        P.op("dve", lambda e: e.tensor_copy(out=qkg_s[:], in_=small["qkg"][:]), reads=[small["qkg"]], writes=[qkg_s])
        P.op("dve", lambda e: e.tensor_scalar_mul(out=qkg_s[:, 0:1], in0=small["qkg"][:, 0:1], scalar1=float(128 ** -0.5)),
             reads=[small["qkg"]], writes=[qkg_s])
        SH1 = lambda: (MOD, 0); G1 = 32; SH2 = 48; G2 = 80

        with P.scope():
            lng_sb = P.sbuf("lng_sb", [128, 1024], F32); lnb_sb = P.sbuf("lnb_sb", [128, 1024], F32)
            sgw_sb = P.sbuf("sgw_sb", [128, 8, 128], BF16); sgb_sb = P.sbuf("sgb_sb", [1, 1024], BF16)
            ones1 = P.sbuf("ones1", [1, 128], BF16); P.op("dve", lambda e: e.memset(ones1[:], 1.0), writes=[ones1])
            P.dma("sp", "ld_lng", lng_sb[:], lng, writes=[lng_sb]); P.dma("sp", "ld_lnb", lnb_sb[:], lnb, writes=[lnb_sb])
            P.dma("pool", "ld_sgw", sgw_sb[:], sgwT, writes=[sgw_sb]); P.dma("pool", "ld_sgb", sgb_sb[:], sgb, writes=[sgb_sb])
            xg = P.sbuf("xg", [128, 16, 512], F32)
            hT = P.sbuf("hT", [128, 16, 512], BF16)
            wp = [P.sbuf(f"wp{i}", [128, 16, 512], BF16) for i in range(3)]
            uT = P.sbuf("uT", [128, 8, 512], BF16)
            z = P.sbuf("z", [128, 4, 1024], F32)
            pfm = [P.psum(f"pfm{i}", [128, 512], F32) for i in range(2)]
            ptm = [P.psum(f"ptm{i}", [128, 512], F32) for i in range(2)]
            pmx = P.psum("pmx", [128, 8, 128], F32)
            stg = [P.sbuf(f"stg{i}", [128, 512], BF16) for i in range(2)]
            vst = [P.sbuf(f"vst{i}", [128, 512], BF16) for i in range(2)]
            zt = P.sbuf("zt", [128, 1024], F32); zsq = P.sbuf("zsq", [128, 1024], BF16); zln = P.sbuf("zln", [128, 1024], BF16)
            st4 = P.sbuf("st4", [128, 8], F32)
            gt = P.sbuf("gt", [128, 8, 128], BF16)
            T1 = norm_tmps(P, "p1", head=True)
            it = 0; ie = 0
            for (t0, TG) in groups(NT):
                s = 0 if t0 >= 256 else 1
                nsub = TG // 128
                P.dma("sp", "ld_xg", xg[:, :, :TG], xT[:, :, t0:t0 + TG], writes=[xg])
                norm_group(P, xg, TG, A1, MOD, 0, s, hT, ones_bf, eps_sb, "n1", T1)
                for pc in range(10):
                    if fz is not None:
                        fz["tick"]()
                    b = wp[it % 3]; key = f"ld_wp{it % 3}"; it += 1
                    for hf in range(2):
                        P.dma("pool", key, b[:, hf * 8:(hf + 1) * 8, :], win[:, hf * 8:(hf + 1) * 8, pc * 512:(pc + 1) * 512], writes=[b])
                    if pc < 4 or pc in (6, 7):
                        for c4 in range(4):
                            ps = pfm[ie % 2]; sg = stg[ie % 2]; ie += 1
                            for kc in range(16):
                                P.op("pe", lambda e: e.matmul(ps[:, :TG], lhsT=b[:, kc, c4 * 128:(c4 + 1) * 128], rhs=hT[:, kc, :TG],
                                                               start=(kc == 0), stop=(kc == 15)), reads=[b, hT], writes=[ps])
                            if pc < 4:
                                hd = (pc % 2) * 4 + c4
                                head_norm(P, ps, TG, qkg_s[:, (pc // 2):(pc // 2) + 1], sg[:, :TG], sg, ones_bf, eps_sb, T1)
                                dst = QT if pc < 2 else KT
                                P.dma("sp", f"st_qk{ie % 2}", dst.t[hd, :, t0:t0 + TG], sg[:, :TG], reads=[sg], writes=[dst])
                            else:
                                ch = (pc - 6) * 4 + c4
                                P.op("act", lambda e: e.activation(out=uT[:, ch, :TG], in_=ps[:, :TG], func=AF.Gelu_apprx_tanh), reads=[ps], writes=[uT])
                    else:
                        for sub in range(nsub):
                            ps = ptm[ie % 2]; vs = vst[ie % 2]; ie += 1
                            for kc in range(16):
                                P.op("pe", lambda e: e.matmul(ps[:], lhsT=hT[:, kc, sub * 128:(sub + 1) * 128], rhs=b[:, kc, :],
                                                               start=(kc == 0), stop=(kc == 15)), reads=[b, hT], writes=[ps])
                            if pc in (4, 5):
                                P.op("act", lambda e: e.activation(out=vs[:], in_=ps[:], func=AF.Copy), reads=[ps], writes=[vs])
                                r0 = t0 + sub * 128
                                P.dma("sp", f"st_v{ie % 2}", V.t[r0:r0 + 128, (pc - 4) * 512:(pc - 3) * 512], vs[:], reads=[vs], writes=[V])
                            else:
                                P.op("act", lambda e: e.activation(out=z[:, sub, (pc - 8) * 512:(pc - 7) * 512], in_=ps[:], func=AF.Gelu_apprx_tanh),
                                     reads=[ps], writes=[z])
                for sub in range(nsub):
                    zz = z[:, sub, :]
                    P.op("dve", lambda e: e.tensor_reduce(out=st4[:, 0:1], in_=zz, axis=AX.X, op=ALU.add), reads=[z], writes=[st4])
                    P.op("dve", lambda e: e.memset(st4[:, 1:2], 0.0), writes=[st4])
                    P.op("act", lambda e: e.activation(out=zsq[:], in_=zz, func=AF.Square, accum_out=st4[:, 1:2]), reads=[z], writes=[zsq, st4])
                    P.op("dve", lambda e: e.tensor_scalar_mul(out=st4[:, 2:3], in0=st4[:, 0:1], scalar1=1.0 / 1024), reads=[st4], writes=[st4])
                    P.op("dve", lambda e: e.tensor_tensor(out=st4[:, 3:4], in0=st4[:, 2:3], in1=st4[:, 2:3], op=ALU.mult), reads=[st4], writes=[st4])
                    P.op("dve", lambda e: e.scalar_tensor_tensor(out=st4[:, 4:5], in0=st4[:, 1:2], scalar=1.0 / 1024, in1=st4[:, 3:4], op0=ALU.mult, op1=ALU.subtract),
                         reads=[st4], writes=[st4])
                    P.op("act", lambda e: e.activation(out=st4[:, 5:6], in_=st4[:, 4:5], func=AF.Sqrt, bias=eps_sb[:, 0:1], scale=1.0), reads=[st4, eps_sb], writes=[st4])
                    P.op("dve", lambda e: e.reciprocal(out=st4[:, 6:7], in_=st4[:, 5:6]), reads=[st4], writes=[st4])
                    P.op("dve", lambda e: e.tensor_scalar(out=zt[:], in0=zz, scalar1=st4[:, 2:3], scalar2=st4[:, 6:7], op0=ALU.subtract, op1=ALU.mult),
                         reads=[z, st4], writes=[zt])
                    P.op("pool", lambda e: e.tensor_tensor(out=zt[:], in0=zt[:], in1=lng_sb[:], op=ALU.mult), reads=[zt, lng_sb], writes=[zt])
                    P.op("pool", lambda e: e.tensor_tensor(out=zln[:], in0=zt[:], in1=lnb_sb[:], op=ALU.add), reads=[zt, lnb_sb], writes=[zln])
                    for g in range(8):
                        P.op("pe", lambda e: e.matmul(pmx[:, g, :], lhsT=zln[:, g * 128:(g + 1) * 128], rhs=sgw_sb[:, g, :], start=True, stop=False),
                             reads=[zln, sgw_sb], writes=[pmx])
                        P.op("pe", lambda e: e.matmul(pmx[:, g, :], lhsT=ones1[:], rhs=sgb_sb[:, g * 128:(g + 1) * 128], start=False, stop=True),
                             reads=[ones1, sgb_sb], writes=[pmx])
                    P.op("dve", lambda e: e.tensor_tensor(out=gt[:], in0=uT[:, :, sub * 128:(sub + 1) * 128], in1=pmx[:], op=ALU.mult),
                         reads=[uT, pmx], writes=[gt])
                    r0 = t0 + sub * 128
                    P.dma("sp", "st_gt", CAT.t[:, 8:16, r0:r0 + 128], gt[:], reads=[gt], writes=[CAT])

        with P.scope():
            nam_sb = P.sbuf("nam_sb", [128, 3200], F32)
            P.dma("sp", "ld_nam", nam_sb[:], nam, writes=[nam_sb])
            bm = P.sbuf("bm", [128, 5, 5, 128], F32)
            qh = P.sbuf("qh", [128, NT], BF16); kh = P.sbuf("kh", [128, NT], BF16); vh = P.sbuf("vh", [128, NTILE, 128], BF16)
            ao = P.sbuf("ao", [128, NT], BF16)
            S = [P.psum(f"S{i}", [128, 8, 128], F32) for i in range(2)]
            Oo = [P.psum(f"Oo{i}", [128, 128], F32) for i in range(2)]
            Dn = [P.psum(f"Dn{i}", [128, 128], F32) for i in range(2)]
            sb = [P.sbuf(f"sb{i}", [128, 5, 128], F32) for i in range(2)]
            pt = [P.sbuf(f"pt{i}", [128, 7, 128], BF16) for i in range(2)]
            rc = [P.sbuf(f"rc{i}", [128, 128], F32) for i in range(2)]
            for h in range(8):
                if fz is not None:
                    fz["tick"]()
                P.dma("sp", "ld_bm", bm[:].rearrange("p a b c -> p (a b c)"), nab[h], writes=[bm])
                P.op("dve", lambda e: e.tensor_tensor(out=bm[:].rearrange("p a b c -> p (a b c)"), in0=bm[:].rearrange("p a b c -> p (a b c)"), in1=nam_sb[:], op=ALU.add),
                     reads=[bm, nam_sb], writes=[bm])
                P.dma("sp", "ld_qh", qh[:], QT.t[h], reads=[QT], writes=[qh])
                P.dma("sp", "ld_kh", kh[:], KT.t[h], reads=[KT], writes=[kh])
                P.dma("sp", "ld_vh", vh[:], V.t[:, h * 128:(h + 1) * 128].rearrange("(n p) d -> p n d", p=128), reads=[V], writes=[vh])
                for qi in range(NTILE):
                    Sx = S[qi % 2]; Ox = Oo[qi % 2]; Dx = Dn[qi % 2]; sbx = sb[qi % 2]; ptx = pt[qi % 2]; rcx = rc[qi % 2]
                    if qi < 2:
                        ktiles = [0, 1]; nloc = 0
                    else:
                        i = qi - 2
                        j0 = min(max(i - 2, 0), 11)
                        cls = 0 if i == 0 else 1 if i == 1 else 3 if i == 14 else 4 if i == 15 else 2
                        ktiles = [2 + j0 + a for a in range(5)] + [0, 1]; nloc = 5
                    nk = len(ktiles)
                    for a, kt in enumerate(ktiles):
                        P.op("pe", lambda e: e.matmul(Sx[:, a, :], lhsT=kh[:, kt * 128:(kt + 1) * 128], rhs=qh[:, qi * 128:(qi + 1) * 128], start=True, stop=True),
                             reads=[kh, qh], writes=[Sx])
                    if nloc:
                        P.op("dve", lambda e: e.tensor_tensor(out=sbx[:], in0=Sx[:, 0:5, :], in1=bm[:, cls, :, :], op=ALU.add), reads=[Sx, bm], writes=[sbx])
                        P.op("act", lambda e: e.activation(out=ptx[:, 0:5, :], in_=sbx[:], func=AF.Exp), reads=[sbx], writes=[ptx])
                    P.op("act", lambda e: e.activation(out=ptx[:, nloc:nk, :], in_=Sx[:, nloc:nk, :], func=AF.Exp), reads=[Sx], writes=[ptx])
                    for a, kt in enumerate(ktiles):
                        P.op("pe", lambda e: e.matmul(Ox[:], lhsT=vh[:, kt, :], rhs=ptx[:, a, :], start=(a == 0), stop=(a == nk - 1)), reads=[vh, ptx], writes=[Ox])
                    for a, kt in enumerate(ktiles):
                        P.op("pe", lambda e: e.matmul(Dx[:], lhsT=ones_bf[:], rhs=ptx[:, a, :], start=(a == 0), stop=(a == nk - 1)), reads=[ones_bf, ptx], writes=[Dx])
                    P.op("dve", lambda e: e.reciprocal(out=rcx[:], in_=Dx[:]), reads=[Dx], writes=[rcx])
                    P.op("dve", lambda e: e.tensor_tensor(out=ao[:, qi * 128:(qi + 1) * 128], in0=Ox[:], in1=rcx[:], op=ALU.mult), reads=[Ox, rcx], writes=[ao])
                P.dma("sp", "st_ao", CAT.t[:, h, :], ao[:], reads=[ao], writes=[CAT])

        with P.scope():
            wo = P.sbuf("wo", [128, 16, 2048], BF16)
            for q4 in range(4):
                P.dma("pool", "ld_wo", wo[:, q4 * 4:(q4 + 1) * 4, :], wout[:, q4 * 4:(q4 + 1) * 4, :], writes=[wo])
            wr_sb = P.sbuf("wr_sb", [128, 16, 32], F32); br_sb = P.sbuf("br_sb", [128, 32], F32)
            P.dma("sp", "ld_wr", wr_sb[:], wr, writes=[wr_sb]); P.dma("sp", "ld_br", br_sb[:], br, writes=[br_sb])
            xg = P.sbuf("xg3", [128, 16, 512], F32); cg = P.sbuf("cg3", [128, 16, 512], BF16)
            h2b = P.sbuf("h2b", [128, 16, 512], BF16); h2f = P.sbuf("h2f", [128, 16, 512], F32)
            py = [P.psum(f"py{i}", [128, 512], F32) for i in range(2)]
            pl = P.psum("pl", [128, 32], F32)
            if fz is not None:
                fz["gp"] = P.psum("gp", [32, 128], F32)
            lg = P.sbuf("lg", [128, 32], F32); mx8 = P.sbuf("mx8", [128, 8], F32)
            idx = P.sbuf("idx", [128, NTILE, 8], U32); gate = P.sbuf("gate", [128, NTILE, 8], F32)
            nm = P.sbuf("nm", [128, 2], F32)
            P.op("dve", lambda e: e.memset(gate[:], 0.0), writes=[gate])
            T3 = norm_tmps(P, "p3")
            for (t0, TG) in groups(NT):
                s = 0 if t0 >= 256 else 1
                P.dma("sp", "ld_xg3", xg[:, :, :TG], xT[:, :, t0:t0 + TG], writes=[xg])
                P.dma("sp", "ld_cg3", cg[:, :, :TG], CAT.t[:, :, t0:t0 + TG], reads=[CAT], writes=[cg])
                for dc in range(16):
                    ps = py[dc % 2]
                    for kc in range(16):
                        P.op("pe", lambda e: e.matmul(ps[:, :TG], lhsT=wo[:, kc, dc * 128:(dc + 1) * 128], rhs=cg[:, kc, :TG], start=(kc == 0), stop=(kc == 15)),
                             reads=[wo, cg], writes=[ps])
                    P.op("dve", lambda e: e.scalar_tensor_tensor(out=xg[:, dc, :TG], in0=ps[:, :TG], scalar=MOD[:, G1 + dc, s:s + 1], in1=xg[:, dc, :TG],
                                                                 op0=ALU.mult, op1=ALU.add), reads=[ps, MOD, xg], writes=[xg])
                P.dma("sp", "st_x1", X1T[:, :, t0:t0 + TG], xg[:, :, :TG], reads=[xg], writes=[X1T])
                norm_group(P, xg, TG, A2, MOD, SH2, s, h2b, ones_bf, eps_sb, "n2", T3, out32=h2f)
                P.dma("sp", "st_h2", H2T[:, :, t0:t0 + TG], h2b[:, :, :TG], reads=[h2b], writes=[H2T])
                for sub in range(TG // 128):
                    ti = (t0 + sub * 128) // 128
                    for kc in range(16):
                        P.op("pe", lambda e: e.matmul(pl[:], lhsT=h2f[:, kc, sub * 128:(sub + 1) * 128], rhs=wr_sb[:, kc, :], start=(kc == 0), stop=(kc == 15)),
                             reads=[h2f, wr_sb], writes=[pl])
                    P.op("dve", lambda e: e.tensor_tensor(out=lg[:], in0=pl[:], in1=br_sb[:], op=ALU.add), reads=[pl, br_sb], writes=[lg])
                    P.op("dve", lambda e: e.max(out=mx8[:], in_=lg[:]), reads=[lg], writes=[mx8])
                    P.op("dve", lambda e: e.max_index(out=idx[:, ti, :], in_max=mx8[:], in_values=lg[:]), reads=[mx8, lg], writes=[idx])
                    P.op("dve", lambda e: e.tensor_scalar_mul(out=nm[:, 0:1], in0=mx8[:, 0:1], scalar1=-1.0), reads=[mx8], writes=[nm])
                    P.op("dve", lambda e: e.memset(nm[:, 1:2], 0.0), writes=[nm])
                    P.op("act", lambda e: e.activation(out=gate[:, ti, 0:4], in_=mx8[:, 0:4], func=AF.Exp, bias=nm[:, 0:1], accum_out=nm[:, 1:2]),
                         reads=[mx8, nm], writes=[gate, nm])
                    P.op("dve", lambda e: e.reciprocal(out=nm[:, 1:2], in_=nm[:, 1:2]), reads=[nm], writes=[nm])
                    P.op("dve", lambda e: e.tensor_scalar_mul(out=gate[:, ti, 0:4], in0=gate[:, ti, 0:4], scalar1=nm[:, 1:2]),
                         reads=[gate, nm], writes=[gate])
                    if fz is not None:
                        gate_matrix(P, fz, lg, mx8, nm, ti)
            P.dma("sp", "st_idx", IDX[:], idx[:], reads=[idx], writes=[IDX])
            P.dma("sp", "st_gate", GATE[:], gate[:], reads=[gate], writes=[GATE])
            if fz is not None:
                P.dma("sp", "st_gt", fz["GT"].t[:, 0:NT], fz["gt_sb"][:, 0:NT], reads=[fz["gt_sb"]], writes=[fz["GT"]])
        if fz is None:
            P.finish(outs)
        print("K1 ninstr", P.ninstr)
    if fz is not None:
        return dict(X1T=X1T, H2T=H2T, MODO=MODO)
    return nc


def pkn(w):
    K, N = w.shape
    return np.ascontiguousarray(w.reshape(K // 128, 128, N).transpose(1, 0, 2))


def pvec(v, n=None):
    return np.ascontiguousarray(v.reshape(-1, 128).T)


def na_tables(rel_bias):
    H = rel_bias.shape[0]
    reps = [0, 1, 2, 14, 15]
    k = np.arange(128); q = np.arange(128)
    nab = np.zeros((H, 128, 5, 5, 128), np.float32); nam = np.zeros((128, 5, 5, 128), np.float32)
    for ci, i in enumerate(reps):
        j0 = min(max(i - 2, 0), 11)
        for a in range(5):
            j = j0 + a
            kr = (2 * j + k // 64)[:, None]; kc = (k % 64)[:, None]
            r = (2 * i + q // 64)[None, :]; qc = (q % 64)[None, :]
            r0 = np.clip(r - 4, 0, 24)
            cs = np.clip(qc - 8, 0, 48)
            valid = (kr >= r0) & (kr < r0 + 8) & (kc >= cs) & (kc < cs + 16)
            ri = np.clip(kr - r + 7, 0, 14); cidx = np.clip(kc - qc + 15, 0, 30)
            nab[:, :, ci, a, :] = rel_bias[:, ri, cidx]
            nam[:, ci, a, :] = np.where(valid, 0.0, -1e30)
    return nab.reshape(H, 128, 3200), nam.reshape(128, 3200)


def k1_inputs(inp, b):
    l = 0
    xcat = np.concatenate([inp["ctx"][b], inp["x"][b]], axis=0)
    nab, nam = na_tables(inp["na_rel_bias"][0])
    return {
        "xT": pkn(np.ascontiguousarray(xcat.T)),
        "cc": np.ascontiguousarray(np.stack([pvec(inp["c"][b]), pvec(inp["c_ctx"])], axis=-1)),
        "adaw": pkn(inp["ada_w"][l]), "adab": pvec(inp["ada_b"][l]),
        "n1g": pvec(inp["norm1_g"][l]), "n2g": pvec(inp["norm2_g"][l]),
        "win": pkn(inp["ab_w_in"][0]), "wout": pkn(inp["ab_w_out"][0]),
        "qkg": np.ascontiguousarray(np.stack([inp["na_q_g"][0], inp["na_k_g"][0]], axis=-1)),
        "nab": nab, "nam": nam,
        "lng": np.ascontiguousarray(np.broadcast_to(inp["sg_norm_g"][0], (128, 1024))),
        "lnb": np.ascontiguousarray(np.broadcast_to(inp["sg_norm_b"][0], (128, 1024))),
        "sgwT": np.ascontiguousarray(inp["sg_w"][0].transpose(2, 0, 1)),
        "sgb": np.ascontiguousarray(inp["sg_b"][0].reshape(1, 1024)),
        "wr": pkn(inp["moe_w_router"][l]), "br": np.ascontiguousarray(np.broadcast_to(inp["moe_b_router"][l], (128, 32))),
    }


CG = 2560
NE = 4


def build_k2(caps):
    nc = bass.Bass("TRN2", target_bir_lowering=False)
    D = lambda name, shape, dt=F32: nc.dram_tensor(name, list(shape), dt, kind="ExternalInput").ap()
    xs = [D(f"xs{j}", [128, 16, caps[j] * 512], BF16) for j in range(NE)]
    wgu = D("wgu", [NE, 128, 16, 4096]); bgu = D("bgu", [128, NE, 32])
    wd = D("wd", [NE, 128, 16, 2048]); bd = D("bd", [128, NE, 16])
    YS = [Buf(nc.dram_tensor(f"ys{j}", [128, 16, caps[j] * 512], F32, kind="ExternalOutput").ap(), f"ys{j}") for j in range(NE)]
    with ExitStack() as st:
        P = Prog(nc, st)
        bgu_sb = P.sbuf("bgu_sb", [128, NE, 32], F32); bd_sb = P.sbuf("bd_sb", [128, NE, 16], F32)
        P.dma("sp", "ld_bgu", bgu_sb[:], bgu, writes=[bgu_sb]); P.dma("sp", "ld_bd", bd_sb[:], bd, writes=[bd_sb])
        xg = [P.sbuf(f"xg{i}", [128, 16, 512], BF16) for i in range(2)]
        hid = P.sbuf("hid", [128, 16, 512], BF16)
        wp = [P.sbuf(f"wp{i}", [128, 16, 512], BF16) for i in range(4)]
        yo = [P.sbuf(f"yo{i}", [128, 4, 512], F32) for i in range(2)]
        pg = [P.psum(f"pg{i}", [128, 512], F32) for i in range(2)]
        pu = [P.psum(f"pu{i}", [128, 512], F32) for i in range(2)]
        py = [P.psum(f"py{i}", [128, 512], F32) for i in range(2)]
        tg = [P.sbuf(f"tg{i}", [128, 512], F32) for i in range(2)]
        ts = [P.sbuf(f"ts{i}", [128, 512], F32) for i in range(2)]
        tu = [P.sbuf(f"tu{i}", [128, 512], F32) for i in range(2)]
        iw = 0; ig = 0; io = 0

        def load_piece(src, e, c0):
            nonlocal iw
            b = wp[iw % 4]; key = f"ld_wp{iw % 4}"; iw += 1
            for hf in range(2):
                P.dma("pool", key, b[:, hf * 8:(hf + 1) * 8, :], src[e, :, hf * 8:(hf + 1) * 8, c0:c0 + 512], writes=[b])
            return b

        for e in range(NE):
            for sg in range(caps[e]):
                x = xg[ig % 2]; ig += 1
                P.dma("sp", f"ld_xg{ig % 2}", x[:], xs[e][:, :, sg * 512:(sg + 1) * 512], writes=[x])
                for f4 in range(4):
                    bg = load_piece(wgu, e, f4 * 512)
                    bu = load_piece(wgu, e, 2048 + f4 * 512)
                    for c4 in range(4):
                        fc = f4 * 4 + c4
                        k = fc % 2
                        for kc in range(16):
                            P.op("pe", lambda en: en.matmul(pg[k][:], lhsT=bg[:, kc, c4 * 128:(c4 + 1) * 128], rhs=x[:, kc, :], start=(kc == 0), stop=(kc == 15)),
                                 reads=[bg, x], writes=[pg[k]])
                        for kc in range(16):
                            P.op("pe", lambda en: en.matmul(pu[k][:], lhsT=bu[:, kc, c4 * 128:(c4 + 1) * 128], rhs=x[:, kc, :], start=(kc == 0), stop=(kc == 15)),
                                 reads=[bu, x], writes=[pu[k]])
                        P.op("dve", lambda en: en.tensor_scalar(out=tg[k][:], in0=pg[k][:], scalar1=bgu_sb[:, e, fc:fc + 1], scalar2=7.0, op0=ALU.add, op1=ALU.min),
                             reads=[pg[k], bgu_sb], writes=[tg[k]])
                        P.op("act", lambda en: en.activation(out=ts[k][:], in_=tg[k][:], func=AF.Sigmoid, scale=1.702), reads=[tg[k]], writes=[ts[k]])
                        P.op("dve", lambda en: en.tensor_scalar(out=tu[k][:], in0=pu[k][:], scalar1=bgu_sb[:, e, 16 + fc:16 + fc + 1], scalar2=7.0, op0=ALU.add, op1=ALU.min),
                             reads=[pu[k], bgu_sb], writes=[tu[k]])
                        P.op("pool", lambda en: en.tensor_scalar(out=tu[k][:], in0=tu[k][:], scalar1=-7.0, scalar2=1.0, op0=ALU.max, op1=ALU.add),
                             reads=[tu[k]], writes=[tu[k]])
                        P.op("pool", lambda en: en.tensor_tensor(out=tg[k][:], in0=tg[k][:], in1=ts[k][:], op=ALU.mult), reads=[tg[k], ts[k]], writes=[tg[k]])
                        P.op("dve", lambda en: en.tensor_tensor(out=hid[:, fc, :], in0=tg[k][:], in1=tu[k][:], op=ALU.mult), reads=[tg[k], tu[k]], writes=[hid])
                for d4 in range(4):
                    bw = load_piece(wd, e, d4 * 512)
                    y = yo[io % 2]; ykey = f"st_y{io % 2}"; io += 1
                    for c4 in range(4):
                        dc = d4 * 4 + c4
                        k = dc % 2
                        for fc in range(16):
                            P.op("pe", lambda en: en.matmul(py[k][:], lhsT=bw[:, fc, c4 * 128:(c4 + 1) * 128], rhs=hid[:, fc, :], start=(fc == 0), stop=(fc == 15)),
                                 reads=[bw, hid], writes=[py[k]])
                        P.op("act", lambda en: en.activation(out=y[:, c4, :], in_=py[k][:], func=AF.Identity, bias=bd_sb[:, e, dc:dc + 1]),
                             reads=[py[k], bd_sb], writes=[y])
                    P.dma("sp", ykey, YS[e][:, d4 * 4:(d4 + 1) * 4, sg * 512:(sg + 1) * 512], y[:], reads=[y], writes=[YS[e]])
        P.finish(YS)
        print("K2 ninstr", P.ninstr)
    return nc


def k2_weights(inp, l, es):
    return {
        "wgu": np.stack([pkn(inp["moe_w_gu"][l, e]) for e in es]),
        "bgu": np.ascontiguousarray(np.stack([inp["moe_b_gu"][l, e].reshape(32, 128).T for e in es], axis=1)),
        "wd": np.stack([pkn(inp["moe_w_down"][l, e]) for e in es]),
        "bd": np.ascontiguousarray(np.stack([inp["moe_b_down"][l, e].reshape(16, 128).T for e in es], axis=1)),
    }


NL = 2048


def combine_group(P, xg, yk, gb, g2buf, g2off, s, TG, tmpc):
    for dc in range(16):
        P.op("dve", lambda e: e.tensor_tensor(out=tmpc[0][:, :TG], in0=yk[0][:, dc, :TG], in1=gb[:, 0, :TG], op=ALU.mult), reads=[yk[0], gb], writes=[tmpc[0]])
        for k in range(1, 4):
            P.op("pool", lambda e: e.tensor_tensor(out=tmpc[1][:, :TG], in0=yk[k][:, dc, :TG], in1=gb[:, k, :TG], op=ALU.mult), reads=[yk[k], gb], writes=[tmpc[1]])
            P.op("dve", lambda e: e.tensor_tensor(out=tmpc[0][:, :TG], in0=tmpc[0][:, :TG], in1=tmpc[1][:, :TG], op=ALU.add), reads=[tmpc[0], tmpc[1]], writes=[tmpc[0]])
        P.op("dve", lambda e: e.scalar_tensor_tensor(out=xg[:, dc, :TG], in0=tmpc[0][:, :TG], scalar=g2buf[:, g2off + dc, s:s + 1], in1=xg[:, dc, :TG],
                                                     op0=ALU.mult, op1=ALU.add), reads=[tmpc[0], g2buf, xg], writes=[xg])


def build_k5():
    nc = bass.Bass("TRN2", target_bir_lowering=False)
    D = lambda name, shape, dt=F32: nc.dram_tensor(name, list(shape), dt, kind="ExternalInput").ap()
    xT = D("xT", [128, 16, NL]); y4 = D("y4", [4, 128, 16, NL]); gb_in = D("gb", [128, 4, NL]); modp = D("modp", [128, 96, 2])
    OUT = Buf(nc.dram_tensor("outT", [128, 16, NL], F32, kind="ExternalOutput").ap(), "outT")
    with ExitStack() as st:
        P = Prog(nc, st)
        MOD = P.sbuf("MOD", [128, 96, 2], F32); P.dma("sp", "ld_mod", MOD[:], modp, writes=[MOD])
        xg = P.sbuf("xg", [128, 16, 512], F32); yk = [P.sbuf(f"yk{k}", [128, 16, 512], F32) for k in range(4)]
        gb = P.sbuf("gbs", [128, 4, 512], F32); tmpc = [P.sbuf(f"tc{i}", [128, 512], F32) for i in range(2)]
        for g in range(4):
            t0 = g * 512
            P.dma("sp", "ld_xg", xg[:], xT[:, :, t0:t0 + 512], writes=[xg])
            P.dma("sp", "ld_gb", gb[:], gb_in[:, :, t0:t0 + 512], writes=[gb])
            for k in range(4):
                P.dma("sp", f"ld_yk{k}", yk[k][:], y4[k, :, :, t0:t0 + 512], writes=[yk[k]])
            combine_group(P, xg, yk, gb, MOD, 80, 0, 512, tmpc)
            P.dma("sp", "st_o", OUT[:, :, t0:t0 + 512], xg[:], reads=[xg], writes=[OUT])
        P.finish([OUT])
    return nc


def build_k4(fz=None):
    pre = "" if fz is None else "b_"
    nc = bass.Bass("TRN2", target_bir_lowering=False) if fz is None else fz["nc"]
    D = lambda name, shape, dt=F32: nc.dram_tensor(pre + name, list(shape), dt, kind="ExternalInput").ap()
    O = (lambda name, shape, dt=F32: nc.dram_tensor(name, list(shape), dt, kind="ExternalOutput").ap()) if fz is None else \
        (lambda name, shape, dt=F32: nc.dram_tensor(pre + name, list(shape), dt).ap())
    I = lambda name, shape, dt=F32: Buf(nc.dram_tensor(pre + name, list(shape), dt).ap(), name)
    if fz is None:
        xT = D("xT", [128, 16, NT]); y4 = D("y4", [4, 128, 16, NT]); gb_in = D("gb", [128, 4, NT]); modp = D("modp", [128, 96, 2])
    cc = D("cc", [128, 16, 2]); adaw = D("adaw", [128, 16, 12288]); adab = D("adab", [128, 96])
    n1g = D("n1g", [128, 16]); n2g = D("n2g", [128, 16])
    win = D("win", [128, 16, 832]); qng = D("qng", [128, 4]); kvng = D("kvng", [128, 2])
    wuq = D("wuq", [128, 4, 3072]); wukv = D("wukv", [128, 2, 4096])
    qg = D("qg", [128, 2]); kg = D("kg", [128, 2])
    cos2 = D("cos2", [64, NL]); sin2 = D("sin2", [64, NL]); pmT = D("pmT", [64, 64])
    wout = D("wout", [128, 16, 2048]); wr = D("wr", [128, 16, 32]); br = D("br", [128, 32])
    X3T = Buf(O("X3T", [128, 16, NL]), "X3T"); H2T = Buf(O("H2T", [128, 16, NL], BF16), "H2T")
    IDX = Buf(O("IDX", [128, 16, 8], U32), "IDX"); GATE = Buf(O("GATE", [128, 16, 8]), "GATE"); MODO = Buf(O("MODO", [128, 96, 2]), "MODO")
    X2 = I("X2", [128, 16, NT]) if fz is None else fz["X2"]
    QN = I("QN", [16, 128, NL], BF16); QR = I("QR", [16, 64, NL], BF16)
    KN = I("KN", [16, 128, NT], BF16); KR = I("KR", [16, 64, NT], BF16); V = I("V", [NT, 2048], BF16); CAT = I("CAT", [128, 16, NL], BF16)
    outs = [X3T, H2T, IDX, GATE, MODO]
    with (ExitStack() if fz is None else fz["P"].scope()) as st:
        P = Prog(nc, st) if fz is None else fz["P"]
        ones_bf = P.sbuf("ones_bf", [128, 128], BF16); P.op("dve", lambda e: e.memset(ones_bf[:], 1.0), writes=[ones_bf])
        eps_sb = P.sbuf("eps_sb", [128, 1], F32); P.op("dve", lambda e: e.memset(eps_sb[:], EPS), writes=[eps_sb])
        if fz is None:
            MOD0 = P.sbuf("MOD0", [128, 96, 2], F32); P.dma("sp", "ld_mod0", MOD0[:], modp, writes=[MOD0])
        MOD = P.sbuf("MOD", [128, 96, 2], F32)
        small = {}
        for name, ap, shp in [("cc", cc, [128, 16, 2]), ("adab", adab, [128, 96]), ("n1g", n1g, [128, 16]), ("n2g", n2g, [128, 16]),
                              ("qng", qng, [128, 4]), ("kvng", kvng, [128, 2]), ("qg", qg, [128, 2]), ("kg", kg, [128, 2])]:
            small[name] = P.sbuf("s_" + name, shp, F32)
            P.dma("sp", "ld_" + name, small[name][:], ap, writes=[small[name]])
        ada_phase(P, nc, adaw, small["adab"], small["cc"], MOD)
        P.dma("sp", "st_mod", MODO[:], MOD[:], reads=[MOD], writes=[MODO])
        A1 = P.sbuf("A1", [128, 16, 2], F32); A2 = P.sbuf("A2", [128, 16, 2], F32)
        for s in range(2):
            for (A, g, off) in ((A1, small["n1g"], 16), (A2, small["n2g"], 64)):
                P.op("dve", lambda e: e.scalar_tensor_tensor(out=A[:, :, s], in0=MOD[:, off:off + 16, s], scalar=1.0, in1=g[:], op0=ALU.add, op1=ALU.mult),
                     reads=[MOD, g], writes=[A])
        qg_s = P.sbuf("qg_s", [128, 2], F32)
        P.op("dve", lambda e: e.tensor_scalar_mul(out=qg_s[:], in0=small["qg"][:], scalar1=float(192 ** -0.5)), reads=[small["qg"]], writes=[qg_s])
        kg_s = small["kg"]
        G1 = 32; SH2 = 48

        with (P.scope() if fz is None else ExitStack()):
          if fz is None:
              xg = P.sbuf("xga", [128, 16, 512], F32); yk = [P.sbuf(f"yk{k}", [128, 16, 512], F32) for k in range(2)]
              gb = P.sbuf("gbs", [128, 4, 512], F32); tmpc = [P.sbuf(f"tc{i}", [128, 512], F32) for i in range(2)]
              for (t0, TG) in groups(NT):
                  s = 0 if t0 >= 256 else 1
                  P.dma("sp", "ld_xga", xg[:, :, :TG], xT[:, :, t0:t0 + TG], writes=[xg])
                  P.dma("sp", "ld_gb", gb[:, :, :TG], gb_in[:, :, t0:t0 + TG], writes=[gb])
                  for kk in range(2):
                      for k in range(2):
                          P.dma("sp", f"ld_yk{k}", yk[k][:, :, :TG], y4[kk * 2 + k, :, :, t0:t0 + TG], writes=[yk[k]])
                      for dc in range(16):
                          P.op("dve", lambda e: e.tensor_tensor(out=tmpc[0][:, :TG], in0=yk[0][:, dc, :TG], in1=gb[:, kk * 2, :TG], op=ALU.mult), reads=[yk[0], gb], writes=[tmpc[0]])
                          P.op("pool", lambda e: e.tensor_tensor(out=tmpc[1][:, :TG], in0=yk[1][:, dc, :TG], in1=gb[:, kk * 2 + 1, :TG], op=ALU.mult), reads=[yk[1], gb], writes=[tmpc[1]])
                          P.op("dve", lambda e: e.tensor_tensor(out=tmpc[0][:, :TG], in0=tmpc[0][:, :TG], in1=tmpc[1][:, :TG], op=ALU.add), reads=[tmpc[0], tmpc[1]], writes=[tmpc[0]])
                          P.op("dve", lambda e: e.scalar_tensor_tensor(out=xg[:, dc, :TG], in0=tmpc[0][:, :TG], scalar=MOD0[:, 80 + dc, s:s + 1], in1=xg[:, dc, :TG],
                                                                       op0=ALU.mult, op1=ALU.add), reads=[tmpc[0], MOD0, xg], writes=[xg])
                  P.dma("sp", "st_x2", X2.t[:, :, t0:t0 + TG], xg[:, :, :TG], reads=[xg], writes=[X2])

        with P.scope():
            win_sb = P.sbuf("win_sb", [128, 16, 832], BF16)
            for hf in range(2):
                P.dma("pool", "ld_win", win_sb[:, hf * 8:(hf + 1) * 8, :], win[:, hf * 8:(hf + 1) * 8, :], writes=[win_sb], max_dma_last_dim=1664)
            wuq_sb = P.sbuf("wuq_sb", [128, 4, 3072], BF16)
            for c in range(4):
                for hf in range(2):
                    P.dma("pool", "ld_wuq", wuq_sb[:, c, hf * 1536:(hf + 1) * 1536], wuq[:, c, hf * 1536:(hf + 1) * 1536], writes=[wuq_sb])
            wukv_sb = P.sbuf("wukv_sb", [128, 2, 4096], BF16)
            for c in range(2):
                for hf in range(2):
                    P.dma("pool", "ld_wukv", wukv_sb[:, c, hf * 2048:(hf + 1) * 2048], wukv[:, c, hf * 2048:(hf + 1) * 2048], writes=[wukv_sb])
            cos_sb = P.sbuf("cos_sb", [64, NL], F32); sin_sb = P.sbuf("sin_sb", [64, NL], F32); pm_sb = P.sbuf("pm_sb", [64, 64], BF16)
            P.dma("sp", "ld_cos", cos_sb[:], cos2, writes=[cos_sb]); P.dma("sp", "ld_sin", sin_sb[:], sin2, writes=[sin_sb])
            P.dma("pool", "ld_pm", pm_sb[:], pmT, writes=[pm_sb])
            xg = P.sbuf("xg", [128, 16, 512], F32)
            hT = P.sbuf("hT", [128, 16, 512], BF16)
            T1 = norm_tmps(P, "p1")
            c32 = P.sbuf("c32", [128, 4, 512], F32); csq = P.sbuf("csq", [128, 4, 512], BF16)
            cqT = P.sbuf("cqT", [128, 4, 512], BF16); ckvT = P.sbuf("ckvT", [128, 2, 512], BF16)
            kpe = P.sbuf("kpe", [64, 512], F32); kpg = P.sbuf("kpg", [64, 512], BF16); kpr = P.sbuf("kpr", [64, 512], F32)
            ksq = P.sbuf("ksq", [64, 512], BF16)
            pA = [P.psum(f"pA{i}", [128, 512], F32) for i in range(2)]
            pB = [P.psum(f"pB{i}", [64, 512], F32) for i in range(2)]
            pS = P.psum("pS", [128, 512], F32)
            pR = P.psum("pR", [64, 512], F32)
            rs = P.sbuf("rs", [128, 512], F32)
            sqn = P.sbuf("sqn", [128, 512], BF16); sqr = P.sbuf("sqr", [64, 512], BF16)
            qr_b = P.sbuf("qr_b", [64, 512], BF16); t1 = P.sbuf("t1", [64, 512], F32); t2 = P.sbuf("t2", [64, 512], F32)
            stn = [P.sbuf(f"stn{i}", [128, 512], BF16) for i in range(2)]
            str_ = [P.sbuf(f"str{i}", [64, 512], BF16) for i in range(2)]
            vst = [P.sbuf(f"vst{i}", [128, 128], BF16) for i in range(2)]
            io = 0

            def rstd_from(ss_ps, TG, scale):
                P.op("act", lambda e: e.activation(out=rs[:, :TG], in_=ss_ps[:, :TG], func=AF.Sqrt, bias=eps_sb[:, 0:1], scale=scale), reads=[ss_ps, eps_sb], writes=[rs])
                P.op("dve", lambda e: e.reciprocal(out=rs[:, :TG], in_=rs[:, :TG]), reads=[rs], writes=[rs])

            def lora_norm(c0, nch, gains, outT, TG):
                for c in range(nch):
                    ps = pA[c % 2]
                    for kc in range(16):
                        P.op("pe", lambda e: e.matmul(ps[:, :TG], lhsT=win_sb[:, kc, c0 + c * 128:c0 + (c + 1) * 128], rhs=hT[:, kc, :TG], start=(kc == 0), stop=(kc == 15)),
                             reads=[win_sb, hT], writes=[ps])
                    P.op("act", lambda e: e.activation(out=c32[:, c, :TG], in_=ps[:, :TG], func=AF.Copy), reads=[ps], writes=[c32])
                    P.op("act", lambda e: e.activation(out=csq[:, c, :TG], in_=ps[:, :TG], func=AF.Square), reads=[ps], writes=[csq])
                for c in range(nch):
                    P.op("pe", lambda e: e.matmul(pS[:, :TG], lhsT=ones_bf[:], rhs=csq[:, c, :TG], start=(c == 0), stop=(c == nch - 1)), reads=[ones_bf, csq], writes=[pS])
                rstd_from(pS, TG, 1.0 / (nch * 128))
                for c in range(nch):
                    P.op("dve", lambda e: e.scalar_tensor_tensor(out=outT[:, c, :TG], in0=c32[:, c, :TG], scalar=gains[:, c:c + 1], in1=rs[:, :TG], op0=ALU.mult, op1=ALU.mult),
                         reads=[c32, gains, rs], writes=[outT])

            def rope(src_b, src_buf, dst, dst_buf, TG, l0):
                P.op("pe", lambda e: e.matmul(pR[:, :TG], lhsT=pm_sb[:], rhs=src_b, start=True, stop=True), reads=[pm_sb, src_buf], writes=[pR])
                P.op("dve", lambda e: e.tensor_tensor(out=t1[:, :TG], in0=src_b, in1=cos_sb[:, l0:l0 + TG], op=ALU.mult), reads=[src_buf, cos_sb], writes=[t1])
                P.op("dve", lambda e: e.tensor_tensor(out=t2[:, :TG], in0=pR[:, :TG], in1=sin_sb[:, l0:l0 + TG], op=ALU.mult), reads=[pR, sin_sb], writes=[t2])
                P.op("dve", lambda e: e.tensor_tensor(out=dst, in0=t1[:, :TG], in1=t2[:, :TG], op=ALU.add), reads=[t1, t2], writes=[dst_buf])

            for (t0, TG) in [(tt, 256) for tt in range(0, NT, 256)]:
                s = 0 if t0 >= 256 else 1
                lat = t0 >= 256
                l0 = t0 - 256
                nsub = TG // 128
                P.dma("sp", "ld_xg", xg[:, :, :TG], X2.t[:, :, t0:t0 + TG], reads=[X2], writes=[xg])
                norm_group(P, xg, TG, A1, MOD, 0, s, hT, ones_bf, eps_sb, "n1", T1)
                lora_norm(512, 2, small["kvng"], ckvT, TG)
                for kc in range(16):
                    P.op("pe", lambda e: e.matmul(pB[0][:, :TG], lhsT=win_sb[:, kc, 768:832], rhs=hT[:, kc, :TG], start=(kc == 0), stop=(kc == 15)), reads=[win_sb, hT], writes=[pB[0]])
                P.op("act", lambda e: e.activation(out=kpe[:, :TG], in_=pB[0][:, :TG], func=AF.Copy), reads=[pB[0]], writes=[kpe])
                P.op("act", lambda e: e.activation(out=ksq[:, :TG], in_=pB[0][:, :TG], func=AF.Square), reads=[pB[0]], writes=[ksq])
                P.op("dve", lambda e: e.tensor_scalar_mul(out=kpg[:, :TG], in0=kpe[:, :TG], scalar1=kg_s[0:64, 1:2]), reads=[kpe, kg_s], writes=[kpg])
                if lat:
                    rope(kpg[:, :TG], kpg, kpr[:, :TG], kpr, TG, l0)
                else:
                    P.op("dve", lambda e: e.tensor_copy(out=kpr[:, :TG], in_=kpg[:, :TG]), reads=[kpg], writes=[kpr])
                for h in range(16):
                    pn = pA[h % 2]
                    for c in range(2):
                        P.op("pe", lambda e: e.matmul(pn[:, :TG], lhsT=wukv_sb[:, c, h * 256:h * 256 + 128], rhs=ckvT[:, c, :TG], start=(c == 0), stop=(c == 1)), reads=[wukv_sb, ckvT], writes=[pn])
                    P.op("act", lambda e: e.activation(out=sqn[:, :TG], in_=pn[:, :TG], func=AF.Square), reads=[pn], writes=[sqn])
                    P.op("pe", lambda e: e.matmul(pS[:, :TG], lhsT=ones_bf[:], rhs=sqn[:, :TG], start=True, stop=False), reads=[ones_bf, sqn], writes=[pS])
                    P.op("pe", lambda e: e.matmul(pS[:, :TG], lhsT=ones_bf[0:64, :], rhs=ksq[:, :TG], start=False, stop=True), reads=[ones_bf, ksq], writes=[pS])
                    rstd_from(pS, TG, 1.0 / 192)
                    sn = stn[io % 2]; sr = str_[io % 2]; io += 1
                    P.op("dve", lambda e: e.scalar_tensor_tensor(out=sn[:, :TG], in0=pn[:, :TG], scalar=kg_s[:, 0:1], in1=rs[:, :TG], op0=ALU.mult, op1=ALU.mult), reads=[pn, kg_s, rs], writes=[sn])
                    P.op("pool", lambda e: e.tensor_tensor(out=sr[:, :TG], in0=kpr[:, :TG], in1=rs[0:64, :TG], op=ALU.mult), reads=[kpr, rs], writes=[sr])
                    P.dma("sp", f"st_kn{io % 2}", KN.t[h, :, t0:t0 + TG], sn[:, :TG], reads=[sn], writes=[KN])
                    P.dma("sp", f"st_kr{io % 2}", KR.t[h, :, t0:t0 + TG], sr[:, :TG], reads=[sr], writes=[KR])
                    for sub in range(nsub):
                        pv = pB[1]
                        vs = vst[(h * 4 + sub) % 2]
                        pvt = pA[(h + 1) % 2]
                        for c in range(2):
                            P.op("pe", lambda e: e.matmul(pvt[:, 0:128], lhsT=ckvT[:, c, sub * 128:(sub + 1) * 128], rhs=wukv_sb[:, c, h * 256 + 128:h * 256 + 256], start=(c == 0), stop=(c == 1)),
                                 reads=[ckvT, wukv_sb], writes=[pvt])
                        P.op("act", lambda e: e.activation(out=vs[:], in_=pvt[:, 0:128], func=AF.Copy), reads=[pvt], writes=[vs])
                        r0 = t0 + sub * 128
                        P.dma("sp", f"st_v{(h * 4 + sub) % 2}", V.t[r0:r0 + 128, h * 128:(h + 1) * 128], vs[:], reads=[vs], writes=[V])
                if not lat:
                    continue
                lora_norm(0, 4, small["qng"], cqT, TG)
                for h in range(16):
                    pn = pA[h % 2]; pr = pB[h % 2]
                    for c in range(4):
                        P.op("pe", lambda e: e.matmul(pn[:, :TG], lhsT=wuq_sb[:, c, h * 192:h * 192 + 128], rhs=cqT[:, c, :TG], start=(c == 0), stop=(c == 3)), reads=[wuq_sb, cqT], writes=[pn])
                    for c in range(4):
                        P.op("pe", lambda e: e.matmul(pr[:, :TG], lhsT=wuq_sb[:, c, h * 192 + 128:h * 192 + 192], rhs=cqT[:, c, :TG], start=(c == 0), stop=(c == 3)), reads=[wuq_sb, cqT], writes=[pr])
                    P.op("act", lambda e: e.activation(out=sqn[:, :TG], in_=pn[:, :TG], func=AF.Square), reads=[pn], writes=[sqn])
                    P.op("act", lambda e: e.activation(out=sqr[:, :TG], in_=pr[:, :TG], func=AF.Square), reads=[pr], writes=[sqr])
                    P.op("pe", lambda e: e.matmul(pS[:, :TG], lhsT=ones_bf[:], rhs=sqn[:, :TG], start=True, stop=False), reads=[ones_bf, sqn], writes=[pS])
                    P.op("pe", lambda e: e.matmul(pS[:, :TG], lhsT=ones_bf[0:64, :], rhs=sqr[:, :TG], start=False, stop=True), reads=[ones_bf, sqr], writes=[pS])
                    rstd_from(pS, TG, 1.0 / 192)
                    sn = stn[io % 2]; sr = str_[io % 2]; io += 1
                    P.op("dve", lambda e: e.scalar_tensor_tensor(out=sn[:, :TG], in0=pn[:, :TG], scalar=qg_s[:, 0:1], in1=rs[:, :TG], op0=ALU.mult, op1=ALU.mult), reads=[pn, qg_s, rs], writes=[sn])
                    P.op("dve", lambda e: e.scalar_tensor_tensor(out=qr_b[:, :TG], in0=pr[:, :TG], scalar=qg_s[0:64, 1:2], in1=rs[0:64, :TG], op0=ALU.mult, op1=ALU.mult), reads=[pr, qg_s, rs], writes=[qr_b])
                    rope(qr_b[:, :TG], qr_b, sr[:, :TG], sr, TG, l0)
                    P.dma("sp", f"st_kn{io % 2}", QN.t[h, :, l0:l0 + TG], sn[:, :TG], reads=[sn], writes=[QN])
                    P.dma("sp", f"st_kr{io % 2}", QR.t[h, :, l0:l0 + TG], sr[:, :TG], reads=[sr], writes=[QR])

        with P.scope():
            qn = P.sbuf("qn", [128, NL], BF16); qr = P.sbuf("qr", [64, NL], BF16)
            kn = P.sbuf("kn", [128, NT], BF16); kr = P.sbuf("kr", [64, NT], BF16)
            vh = P.sbuf("vh", [128, 18, 128], BF16); ao = P.sbuf("ao", [128, NL], BF16)
            S = [P.psum(f"S{i}", [128, 512], F32) for i in range(2)]
            Oo = P.psum("Oo", [128, 512], F32); Dn = P.psum("Dn", [128, 512], F32)
            pt = [P.sbuf(f"pt{i}", [128, 512], BF16) for i in range(2)]
            rc = P.sbuf("rc", [128, 512], F32)
            for h in range(16):
                P.dma("sp", "ld_qn", qn[:], QN.t[h], reads=[QN], writes=[qn]); P.dma("sp", "ld_qr", qr[:], QR.t[h], reads=[QR], writes=[qr])
                P.dma("sp", "ld_kn", kn[:], KN.t[h], reads=[KN], writes=[kn]); P.dma("sp", "ld_kr", kr[:], KR.t[h], reads=[KR], writes=[kr])
                P.dma("sp", "ld_vh", vh[:], V.t[:, h * 128:(h + 1) * 128].rearrange("(n p) d -> p n d", p=128), reads=[V], writes=[vh])
                for qg4 in range(4):
                    q0 = qg4 * 512
                    for kt in range(18):
                        Sx = S[kt % 2]; px = pt[kt % 2]
                        P.op("pe", lambda e: e.matmul(Sx[:], lhsT=kn[:, kt * 128:(kt + 1) * 128], rhs=qn[:, q0:q0 + 512], start=True, stop=False), reads=[kn, qn], writes=[Sx])
                        P.op("pe", lambda e: e.matmul(Sx[:], lhsT=kr[:, kt * 128:(kt + 1) * 128], rhs=qr[:, q0:q0 + 512], start=False, stop=True), reads=[kr, qr], writes=[Sx])
                        P.op("act", lambda e: e.activation(out=px[:], in_=Sx[:], func=AF.Exp), reads=[Sx], writes=[px])
                        P.op("pe", lambda e: e.matmul(Oo[:], lhsT=vh[:, kt, :], rhs=px[:], start=(kt == 0), stop=(kt == 17)), reads=[vh, px], writes=[Oo])
                        P.op("pe", lambda e: e.matmul(Dn[:], lhsT=ones_bf[:], rhs=px[:], start=(kt == 0), stop=(kt == 17)), reads=[ones_bf, px], writes=[Dn])
                    P.op("dve", lambda e: e.reciprocal(out=rc[:], in_=Dn[:]), reads=[Dn], writes=[rc])
                    P.op("dve", lambda e: e.tensor_tensor(out=ao[:, q0:q0 + 512], in0=Oo[:], in1=rc[:], op=ALU.mult), reads=[Oo, rc], writes=[ao])
                P.dma("sp", "st_ao", CAT.t[:, h, :], ao[:], reads=[ao], writes=[CAT])

        with P.scope():
            wo = P.sbuf("wo", [128, 16, 2048], BF16)
            for q4 in range(4):
                P.dma("pool", "ld_wo", wo[:, q4 * 4:(q4 + 1) * 4, :], wout[:, q4 * 4:(q4 + 1) * 4, :], writes=[wo])
            wr_sb = P.sbuf("wr_sb", [128, 16, 32], F32); br_sb = P.sbuf("br_sb", [128, 32], F32)
            P.dma("sp", "ld_wr", wr_sb[:], wr, writes=[wr_sb]); P.dma("sp", "ld_br", br_sb[:], br, writes=[br_sb])
            xg = P.sbuf("xg3", [128, 16, 512], F32); cg = P.sbuf("cg3", [128, 16, 512], BF16)
            h2b = P.sbuf("h2b", [128, 16, 512], BF16); h2f = P.sbuf("h2f", [128, 16, 512], F32)
            py = [P.psum(f"py{i}", [128, 512], F32) for i in range(2)]
            pl = P.psum("pl", [128, 32], F32)
            if fz is not None:
                fz["gp"] = P.psum("gp", [32, 128], F32)
            lg = P.sbuf("lg", [128, 32], F32); mx8 = P.sbuf("mx8", [128, 8], F32)
            idx = P.sbuf("idx", [128, 16, 8], U32); gate = P.sbuf("gate", [128, 16, 8], F32)
            nm = P.sbuf("nm", [128, 2], F32)
            P.op("dve", lambda e: e.memset(gate[:], 0.0), writes=[gate])
            T3 = norm_tmps(P, "p3")
            for g4 in range(4):
                t0 = g4 * 512; TG = 512; s = 0
                P.dma("sp", "ld_xg3", xg[:], X2.t[:, :, 256 + t0:256 + t0 + TG], reads=[X2], writes=[xg])
                P.dma("sp", "ld_cg3", cg[:], CAT.t[:, :, t0:t0 + TG], reads=[CAT], writes=[cg])
                for dc in range(16):
                    ps = py[dc % 2]
                    for kc in range(16):
                        P.op("pe", lambda e: e.matmul(ps[:], lhsT=wo[:, kc, dc * 128:(dc + 1) * 128], rhs=cg[:, kc, :], start=(kc == 0), stop=(kc == 15)), reads=[wo, cg], writes=[ps])
                    P.op("dve", lambda e: e.scalar_tensor_tensor(out=xg[:, dc, :], in0=ps[:], scalar=MOD[:, G1 + dc, 0:1], in1=xg[:, dc, :], op0=ALU.mult, op1=ALU.add), reads=[ps, MOD, xg], writes=[xg])
                P.dma("sp", "st_x3", X3T[:, :, t0:t0 + TG], xg[:], reads=[xg], writes=[X3T])
                norm_group(P, xg, TG, A2, MOD, SH2, 0, h2b, ones_bf, eps_sb, "n2", T3, out32=h2f)
                P.dma("sp", "st_h2", H2T[:, :, t0:t0 + TG], h2b[:], reads=[h2b], writes=[H2T])
                for sub in range(4):
                    ti = g4 * 4 + sub
                    for kc in range(16):
                        P.op("pe", lambda e: e.matmul(pl[:], lhsT=h2f[:, kc, sub * 128:(sub + 1) * 128], rhs=wr_sb[:, kc, :], start=(kc == 0), stop=(kc == 15)), reads=[h2f, wr_sb], writes=[pl])
                    P.op("dve", lambda e: e.tensor_tensor(out=lg[:], in0=pl[:], in1=br_sb[:], op=ALU.add), reads=[pl, br_sb], writes=[lg])
                    P.op("dve", lambda e: e.max(out=mx8[:], in_=lg[:]), reads=[lg], writes=[mx8])
                    P.op("dve", lambda e: e.max_index(out=idx[:, ti, :], in_max=mx8[:], in_values=lg[:]), reads=[mx8, lg], writes=[idx])
                    P.op("dve", lambda e: e.tensor_scalar_mul(out=nm[:, 0:1], in0=mx8[:, 0:1], scalar1=-1.0), reads=[mx8], writes=[nm])
                    P.op("dve", lambda e: e.memset(nm[:, 1:2], 0.0), writes=[nm])
                    P.op("act", lambda e: e.activation(out=gate[:, ti, 0:4], in_=mx8[:, 0:4], func=AF.Exp, bias=nm[:, 0:1], accum_out=nm[:, 1:2]), reads=[mx8, nm], writes=[gate, nm])
                    P.op("dve", lambda e: e.reciprocal(out=nm[:, 1:2], in_=nm[:, 1:2]), reads=[nm], writes=[nm])
                    P.op("dve", lambda e: e.tensor_scalar_mul(out=gate[:, ti, 0:4], in0=gate[:, ti, 0:4], scalar1=nm[:, 1:2]), reads=[gate, nm], writes=[gate])
                    if fz is not None:
                        gate_matrix(P, fz, lg, mx8, nm, ti)
            P.dma("sp", "st_idx", IDX[:], idx[:], reads=[idx], writes=[IDX])
            P.dma("sp", "st_gate", GATE[:], gate[:], reads=[gate], writes=[GATE])
            if fz is not None:
                P.dma("sp", "st_gt", fz["GT"].t[:, 0:NL], fz["gt_sb"][:, 0:NL], reads=[fz["gt_sb"]], writes=[fz["GT"]])
        if fz is None:
            P.finish(outs)
        print("K4 ninstr", P.ninstr)
    if fz is not None:
        return dict(X3T=X3T, H2T=H2T, MODO=MODO)
    return nc


def cast_jobs(P, wsrc, W16, ncols, key):
    jobs = []
    for e in range(32):
        W = W16[e // 8]
        for kc in range(16):
            for c0 in range(0, ncols, 2048):
                pc0 = c0 // 512
                jobs.append(lambda W=W, e=e, kc=kc, c0=c0, pc0=pc0: P.dma(
                    "pool", key, W.t[e % 8, pc0:pc0 + 4, :, kc, :].rearrange("pc p n -> p pc n"),
                    wsrc[e, kc * 128:(kc + 1) * 128, c0:c0 + 2048].rearrange("p (pc n) -> p pc n", n=512), writes=[W]))
    return jobs


def moe_dense(P, nc, l, grp, Xin, H2, GT, Xout, MODO, WGU16, WD16, bgu, bd, sel):
    with P.scope():
        mod = P.sbuf("mmod", [128, 96, 2], F32); P.dma("sp", "ld_mmod", mod[:], MODO[:], reads=[MODO], writes=[mod])
        bgu_sb = P.sbuf("mbgu", [128, 32, 32], F32); bd_sb = P.sbuf("mbd", [128, 32, 16], F32)
        P.dma("sp", "ld_mbgu", bgu_sb[:], bgu[:, l], writes=[bgu_sb]); P.dma("sp", "ld_mbd", bd_sb[:], bd[:, l], writes=[bd_sb])
        sel_sb = P.sbuf("msel", [32, 32, 128], F32); P.dma("sp", "ld_msel", sel_sb[:], sel, writes=[sel_sb])
        gtg = P.sbuf("mgtg", [32, 512], F32)
        xg = P.sbuf("mxg", [128, 16, 512], F32); hg = P.sbuf("mhg", [128, 16, 512], BF16); hid = P.sbuf("mhid", [128, 16, 512], BF16)
        wp = [P.sbuf(f"mwp{i}", [128, 16, 512], BF16) for i in range(4)]
        gbs = P.sbuf("mgbs", [128, 512], F32)
        pg = [P.psum(f"mpg{i}", [128, 512], F32) for i in range(2)]
        pu = [P.psum(f"mpu{i}", [128, 512], F32) for i in range(2)]
        py = [P.psum(f"mpy{i}", [128, 512], F32) for i in range(2)]
        pgb = P.psum("mpgb", [128, 512], F32)
        tg = [P.sbuf(f"mtg{i}", [128, 512], F32) for i in range(2)]
        ts = [P.sbuf(f"mts{i}", [128, 512], F32) for i in range(2)]
        tu = [P.sbuf(f"mtu{i}", [128, 512], F32) for i in range(2)]
        ty = [P.sbuf(f"mty{i}", [128, 512], F32) for i in range(2)]
        iw = 0

        def load_piece(W, e, c0):
            nonlocal iw
            b = wp[iw % 4]; key = f"ld_mwp{iw % 4}"; iw += 1
            Wb = W[e // 8]
            P.dma("sp", key, b[:], Wb.t[e % 8, c0 // 512], reads=[Wb], writes=[b])
            return b

        for (t0, TG) in grp:
            s = 1 if (Xin.t.shape[2] == 2304 and t0 < 256) else 0
            P.dma("sp", "ld_mxg", xg[:, :, :TG], Xin.t[:, :, t0:t0 + TG], reads=[Xin], writes=[xg])
            P.dma("sp", "ld_mhg", hg[:, :, :TG], H2.t[:, :, t0:t0 + TG], reads=[H2], writes=[hg])
            P.dma("sp", "ld_mgt", gtg[:, :TG], GT.t[:, t0:t0 + TG], reads=[GT], writes=[gtg])
            for e in range(32):
                P.op("pe", lambda en: en.matmul(pgb[:, :TG], lhsT=sel_sb[:, e, :], rhs=gtg[:, :TG], start=True, stop=True), reads=[sel_sb, gtg], writes=[pgb])
                P.op("act", lambda en: en.activation(out=gbs[:, :TG], in_=pgb[:, :TG], func=AF.Copy), reads=[pgb], writes=[gbs])
                for f4 in range(4):
                    bg = load_piece(WGU16, e, f4 * 512)
                    bu = load_piece(WGU16, e, 2048 + f4 * 512)
                    for c4 in range(4):
                        fc = f4 * 4 + c4
                        k = fc % 2
                        for kc in range(16):
                            P.op("pe", lambda en: en.matmul(pg[k][:, :TG], lhsT=bg[:, kc, c4 * 128:(c4 + 1) * 128], rhs=hg[:, kc, :TG], start=(kc == 0), stop=(kc == 15)),
                                 reads=[bg, hg], writes=[pg[k]], signal=(kc == 15))
                        for kc in range(16):
                            P.op("pe", lambda en: en.matmul(pu[k][:, :TG], lhsT=bu[:, kc, c4 * 128:(c4 + 1) * 128], rhs=hg[:, kc, :TG], start=(kc == 0), stop=(kc == 15)),
                                 reads=[bu, hg], writes=[pu[k]], signal=(kc == 15))
                        P.op("dve", lambda en: en.tensor_scalar(out=tg[k][:, :TG], in0=pg[k][:, :TG], scalar1=bgu_sb[:, e, fc:fc + 1], scalar2=7.0, op0=ALU.add, op1=ALU.min),
                             reads=[pg[k], bgu_sb], writes=[tg[k]])
                        P.op("act", lambda en: en.activation(out=ts[k][:, :TG], in_=tg[k][:, :TG], func=AF.Sigmoid, scale=1.702), reads=[tg[k]], writes=[ts[k]])
                        P.op("dve", lambda en: en.tensor_scalar(out=tu[k][:, :TG], in0=pu[k][:, :TG], scalar1=bgu_sb[:, e, 16 + fc:16 + fc + 1], scalar2=7.0, op0=ALU.add, op1=ALU.min),
                             reads=[pu[k], bgu_sb], writes=[tu[k]])
                        P.op("dve", lambda en: en.tensor_scalar(out=tu[k][:, :TG], in0=tu[k][:, :TG], scalar1=-7.0, scalar2=1.0, op0=ALU.max, op1=ALU.add),
                             reads=[tu[k]], writes=[tu[k]])
                        P.op("dve", lambda en: en.tensor_tensor(out=tg[k][:, :TG], in0=tg[k][:, :TG], in1=ts[k][:, :TG], op=ALU.mult), reads=[tg[k], ts[k]], writes=[tg[k]])
                        P.op("dve", lambda en: en.tensor_tensor(out=hid[:, fc, :TG], in0=tg[k][:, :TG], in1=tu[k][:, :TG], op=ALU.mult), reads=[tg[k], tu[k]], writes=[hid])
                for d4 in range(4):
                    bw = load_piece(WD16, e, d4 * 512)
                    for c4 in range(4):
                        dc = d4 * 4 + c4
                        k = dc % 2
                        for fc in range(16):
                            P.op("pe", lambda en: en.matmul(py[k][:, :TG], lhsT=bw[:, fc, c4 * 128:(c4 + 1) * 128], rhs=hid[:, fc, :TG], start=(fc == 0), stop=(fc == 15)),
                                 reads=[bw, hid], writes=[py[k]], signal=(fc == 15))
                        P.op("dve", lambda en: en.scalar_tensor_tensor(out=ty[k][:, :TG], in0=py[k][:, :TG], scalar=bd_sb[:, e, dc:dc + 1], in1=gbs[:, :TG], op0=ALU.add, op1=ALU.mult),
                             reads=[py[k], bd_sb, gbs], writes=[ty[k]])
                        P.op("dve", lambda en: en.scalar_tensor_tensor(out=xg[:, dc, :TG], in0=ty[k][:, :TG], scalar=mod[:, 80 + dc, s:s + 1], in1=xg[:, dc, :TG], op0=ALU.mult, op1=ALU.add),
                             reads=[ty[k], mod, xg], writes=[xg])
            P.dma("sp", "st_mx", Xout.t[:, :, t0:t0 + TG], xg[:, :, :TG], reads=[xg], writes=[Xout])


def build_fused():
    nc = bass.Bass("TRN2", target_bir_lowering=False)
    D = lambda name, shape, dt=F32: nc.dram_tensor(name, list(shape), dt, kind="ExternalInput").ap()
    I = lambda name, shape, dt=F32: Buf(nc.dram_tensor(name, list(shape), dt).ap(), name)
    wgu = [D(f"wgu{l}", [32, 2048, 4096]) for l in range(2)]; wd = [D(f"wd{l}", [32, 2048, 2048]) for l in range(2)]
    bgu = D("mbgu_in", [128, 2, 32, 32]); bd = D("mbd_in", [128, 2, 32, 16]); sel = D("msel_in", [32, 32, 128]); ident_in = D("ident_in", [128, 128])
    OUT = Buf(nc.dram_tensor("outT", [128, 16, NL], F32, kind="ExternalOutput").ap(), "outT")
    WGU16 = [[I(f"WGU16_{l}_{q}", [8, 8, 128, 16, 512], BF16) for q in range(4)] for l in range(2)]
    WD16 = [[I(f"WD16_{l}_{q}", [8, 4, 128, 16, 512], BF16) for q in range(4)] for l in range(2)]
    GT = [I("GT0", [32, NT]), I("GT1", [32, NL])]; X2 = I("X2f", [128, 16, NT])
    with ExitStack() as st:
        P = Prog(nc, st)
        ident = P.sbuf("ident", [128, 128], F32); P.dma("sp", "ld_ident", ident[:], ident_in, writes=[ident])
        fz = {"nc": nc, "P": P, "ident": ident, "gm": P.sbuf("gm", [128, 32], F32), "gx": P.sbuf("gx", [128, 32], F32),
              "gt_sb": P.sbuf("gt_sb", [32, NT], F32), "GT": GT[0], "X2": X2}
        jobs0 = cast_jobs(P, wgu[0], WGU16[0], 4096, "cast0") + cast_jobs(P, wd[0], WD16[0], 2048, "cast0")
        NTICK = 60
        per = -(-len(jobs0) // NTICK)

        def tick():
            for _ in range(per):
                if jobs0:
                    jobs0.pop(0)()
        fz["tick"] = tick
        r1 = build_k1(fz)
        while jobs0:
            jobs0.pop(0)()
        fz["tick"] = lambda: None
        for j in cast_jobs(P, wgu[1], WGU16[1], 4096, "cast1") + cast_jobs(P, wd[1], WD16[1], 2048, "cast1"):
            j()
        moe_dense(P, nc, 0, groups(NT), r1["X1T"], r1["H2T"], GT[0], X2, r1["MODO"], WGU16[0], WD16[0], bgu, bd, sel)
        fz["GT"] = GT[1]
        r4 = build_k4(fz)
        moe_dense(P, nc, 1, [(t, 512) for t in range(0, NL, 512)], r4["X3T"], r4["H2T"], GT[1], OUT, r4["MODO"], WGU16[1], WD16[1], bgu, bd, sel)
        P.finish([OUT])
        print("FUSED ninstr", P.ninstr)
    return nc


def fused_inputs(inp, b):
    d = {"a_" + k: v for k, v in k1_inputs(inp, b).items()}
    cos2, sin2, pmT = _rope_consts()
    l = 1
    d.update({
        "b_cc": d["a_cc"], "b_adaw": pkn(inp["ada_w"][l]), "b_adab": pvec(inp["ada_b"][l]), "b_n1g": pvec(inp["norm1_g"][l]), "b_n2g": pvec(inp["norm2_g"][l]),
        "b_win": pkn(inp["mla_w_in"][0]), "b_qng": pvec(inp["mla_q_norm_g"][0]), "b_kvng": pvec(inp["mla_kv_norm_g"][0]),
        "b_wuq": pkn(inp["mla_w_uq"][0]), "b_wukv": pkn(inp["mla_w_ukv"][0]),
        "b_qg": _pad_gain(inp["mla_q_g"][0]), "b_kg": _pad_gain(inp["mla_k_g"][0]),
        "b_cos2": cos2, "b_sin2": sin2, "b_pmT": pmT, "b_wout": pkn(inp["mla_w_out"][0]),
        "b_wr": pkn(inp["moe_w_router"][l]), "b_br": np.ascontiguousarray(np.broadcast_to(inp["moe_b_router"][l], (128, 32))),
        "wgu0": inp["moe_w_gu"][0], "wgu1": inp["moe_w_gu"][1], "wd0": inp["moe_w_down"][0], "wd1": inp["moe_w_down"][1],
        "mbgu_in": np.ascontiguousarray(inp["moe_b_gu"].reshape(2, 32, 32, 128).transpose(3, 0, 1, 2)),
        "mbd_in": np.ascontiguousarray(inp["moe_b_down"].reshape(2, 32, 16, 128).transpose(3, 0, 1, 2)),
        "msel_in": np.ascontiguousarray(np.broadcast_to(np.eye(32, dtype=np.float32)[:, :, None], (32, 32, 128))),
        "ident_in": np.eye(128, dtype=np.float32),
    })
    return d


def kernel_fused_1core(inp, b):
    nc = build_fused()
    res = run_bass_kernel_spmd(nc, [fused_inputs(inp, b)], core_ids=[0])
    return _tok_major(np.asarray(res.results[0]["outT"]))


NCORES = 8


def _run(nc, in_maps):
    res = run_bass_kernel_spmd(nc, in_maps, core_ids=list(range(NCORES)))
    return res.results


def _tok_major(a):
    return np.ascontiguousarray(a.transpose(2, 1, 0).reshape(a.shape[2], 2048))


def _feat_major(a):
    T = a.shape[-2]
    lead = a.shape[:-2]
    b = a.reshape(*lead, T, 16, 128)
    n = len(lead)
    return np.ascontiguousarray(b.transpose(*range(n), n + 2, n + 1, n))


def _moe_route_and_run(inp, l, res, T):
    h2 = [_tok_major(np.asarray(r["H2T"])) for r in res]
    idx = [np.asarray(r["IDX"]).transpose(1, 0, 2).reshape(T, 8)[:, :4].astype(np.int64) for r in res]
    gate = [np.asarray(r["GATE"]).transpose(1, 0, 2).reshape(T, 8)[:, :4] for r in res]
    e_flat = np.concatenate([i.reshape(-1) for i in idx])
    order = np.argsort(e_flat, kind="stable")
    counts = np.bincount(e_flat, minlength=32)
    starts = np.cumsum(counts) - counts
    pos = np.empty_like(e_flat)
    pos[order] = np.arange(e_flat.size) - starts[e_flat[order]]
    ng = np.maximum(1, -(-counts // 512))
    rank = np.argsort(-ng, kind="stable")
    caps = [int(ng[rank[8 * j]]) for j in range(NE)]
    nc2 = build_k2(caps)
    h2_all = np.concatenate(h2, axis=0)
    tok_of = np.arange(e_flat.size) // 4
    ins = []
    for c in range(NCORES):
        es = [int(rank[8 * j + c]) for j in range(NE)]
        d = k2_weights(inp, l, es)
        for j, e in enumerate(es):
            xs = np.zeros((caps[j] * 512, 2048), dtype=h2_all.dtype)
            sel = np.nonzero(e_flat == e)[0]
            xs[pos[sel]] = h2_all[tok_of[sel]]
            d[f"xs{j}"] = _feat_major(xs)
        ins.append(d)
    r2 = _run(nc2, ins)
    y_assign = np.zeros((e_flat.size, 2048), np.float32)
    for c in range(NCORES):
        for j in range(NE):
            e = int(rank[8 * j + c])
            ys = _tok_major(np.asarray(r2[c][f"ys{j}"]))
            sel = np.nonzero(e_flat == e)[0]
            y_assign[sel] = ys[pos[sel]]
    y_assign = y_assign.reshape(NCORES, T, 4, 2048)
    out = []
    for b in range(NCORES):
        y4 = _feat_major(np.ascontiguousarray(y_assign[b].transpose(1, 0, 2)))
        gbc = np.ascontiguousarray(np.broadcast_to(gate[b].T[None], (128, 4, T))).astype(np.float32)
        out.append((y4, gbc))
    return out


def _rope_consts():
    t = np.arange(2048)
    row = (t // 64).astype(np.float32); col = (t % 64).astype(np.float32)
    inv = (np.float32(10000.0) ** (-np.arange(16, dtype=np.float32) / np.float32(16))).astype(np.float32)
    ang = np.concatenate([row[:, None] * inv, col[:, None] * inv], axis=-1).astype(np.float32)
    cos = np.cos(ang).astype(np.float32); sin = np.sin(ang).astype(np.float32)
    cos2 = np.ascontiguousarray(np.concatenate([cos.T, cos.T], axis=0)); sin2 = np.ascontiguousarray(np.concatenate([sin.T, sin.T], axis=0))
    pmT = np.zeros((64, 64), np.float32)
    for m in range(32):
        pmT[m + 32, m] = -1.0
    for m in range(32, 64):
        pmT[m - 32, m] = 1.0
    return cos2, sin2, pmT


def _pad_gain(g):
    out = np.zeros((128, 2), np.float32)
    out[:, 0] = g[:128]
    out[:64, 1] = g[128:192]
    return out


def kernel_unfused(**inp):
    inp = {k: np.asarray(v) for k, v in inp.items()}
    nc1 = build_k1()
    r1 = _run(nc1, [k1_inputs(inp, b) for b in range(NCORES)])
    m0 = _moe_route_and_run(inp, 0, r1, 2304)
    cos2, sin2, pmT = _rope_consts()
    l = 1
    shared = {
        "adaw": pkn(inp["ada_w"][l]), "adab": pvec(inp["ada_b"][l]), "n1g": pvec(inp["norm1_g"][l]), "n2g": pvec(inp["norm2_g"][l]),
        "win": pkn(inp["mla_w_in"][0]), "qng": pvec(inp["mla_q_norm_g"][0]), "kvng": pvec(inp["mla_kv_norm_g"][0]),
        "wuq": pkn(inp["mla_w_uq"][0]), "wukv": pkn(inp["mla_w_ukv"][0]),
        "qg": _pad_gain(inp["mla_q_g"][0]), "kg": _pad_gain(inp["mla_k_g"][0]),
        "cos2": cos2, "sin2": sin2, "pmT": pmT, "wout": pkn(inp["mla_w_out"][0]),
        "wr": pkn(inp["moe_w_router"][l]), "br": np.ascontiguousarray(np.broadcast_to(inp["moe_b_router"][l], (128, 32))),
    }
    ins4 = []
    for b in range(NCORES):
        d = dict(shared)
        d["xT"] = np.asarray(r1[b]["X1T"]); d["y4"] = m0[b][0]; d["gb"] = m0[b][1]; d["modp"] = np.asarray(r1[b]["MODO"])
        d["cc"] = np.ascontiguousarray(np.stack([pvec(inp["c"][b]), pvec(inp["c_ctx"])], axis=-1))
        ins4.append(d)
    nc4 = build_k4()
    r4 = _run(nc4, ins4)
    del m0, ins4
    m1 = _moe_route_and_run(inp, 1, r4, 2048)
    nc5 = build_k5()
    ins5 = [{"xT": np.asarray(r4[b]["X3T"]), "y4": m1[b][0], "gb": m1[b][1], "modp": np.asarray(r4[b]["MODO"])} for b in range(NCORES)]
    r5 = _run(nc5, ins5)
    out = np.stack([_tok_major(np.asarray(r5[b]["outT"])) for b in range(NCORES)], axis=0).astype(np.float32)
    return out


def kernel(**inp):
    inp = {k: np.asarray(v) for k, v in inp.items()}
    nc = build_fused()
    shared = fused_inputs(inp, 0)
    ins = []
    for b in range(NCORES):
        d = dict(shared)
        xcat = np.concatenate([inp["ctx"][b], inp["x"][b]], axis=0)
        d["a_xT"] = pkn(np.ascontiguousarray(xcat.T))
        cc = np.ascontiguousarray(np.stack([pvec(inp["c"][b]), pvec(inp["c_ctx"])], axis=-1))
        d["a_cc"] = cc; d["b_cc"] = cc
        ins.append(d)
    res = _run(nc, ins)
    return np.stack([_tok_major(np.asarray(res[b]["outT"])) for b in range(NCORES)], axis=0).astype(np.float32)
```
